# Optimizing a Trainium2 kernel written in Bass

```python
import math
import jax, jax.numpy as jnp
from jax import lax
import numpy as np

D_MODEL = 1024
BATCH = 8
SEQ = 2048
DEPTH = 1

HEAD_DIM = 64
N_HEADS_A = 8
N_HEADS_B = 8
D_A = N_HEADS_A * HEAD_DIM
D_B = N_HEADS_B * HEAD_DIM
D_MIX = D_A + D_B
N_IN = 3 * D_A + 3 * D_B + N_HEADS_B
DILATED_PATTERNS = ((128, 1), (512, 4), (2048, 16))
ROPE_THETA = 500000.0
ROT_DIM = HEAD_DIM // 4
Q_BLOCK = 128
N_GROUPS = 4
EXPERTS_PER_GROUP = 8
N_EXPERTS = N_GROUPS * EXPERTS_PER_GROUP
TOP_K = 2
D_EXPERT = 512
PLE_DIM = 256
EPS = 1e-6
NEG = -1e30
FORGET_BIAS = 2.5

kernel_name = "hymba_dilated_fox_hmoe_block"


def rmsnorm(x, g):
    xf = x.astype(jnp.float32)
    y = xf * lax.rsqrt(jnp.mean(xf * xf, axis=-1, keepdims=True) + EPS)
    return (y * g.astype(jnp.float32)).astype(x.dtype)


def rope_tables(positions):
    pos = positions.astype(jnp.float32)
    inv = ROPE_THETA ** (-jnp.arange(0, ROT_DIM, 2, dtype=jnp.float32) / ROT_DIM)
    ang = pos[..., None] * inv
    return jnp.cos(ang)[:, :, None, :], jnp.sin(ang)[:, :, None, :]


def partial_rope(t, cos, sin):
    half = ROT_DIM // 2
    tf = t.astype(jnp.float32)
    t1, t2, rest = tf[..., :half], tf[..., half:ROT_DIM], tf[..., ROT_DIM:]
    out = jnp.concatenate([t1 * cos - t2 * sin, t2 * cos + t1 * sin, rest], axis=-1)
    return out.astype(t.dtype)


def dilated_window_attention(q, k, v, window, dilation):
    B, S, H, Dh = q.shape
    d = dilation
    W = window // d
    L = S // d
    nb = -(-L // W)
    pad = nb * W - L

    def to_blocks(a):
        a = a.reshape(B, L, d, H, Dh).transpose(0, 2, 1, 3, 4).reshape(B * d, L, H, Dh)
        a = jnp.pad(a, ((0, 0), (0, pad), (0, 0), (0, 0)))
        return a.reshape(B * d, nb, W, H, Dh)

    def with_prev(a):
        prev = jnp.pad(a, ((0, 0), (1, 0), (0, 0), (0, 0), (0, 0)))[:, :-1]
        return jnp.concatenate([prev, a], axis=2)

    qb, kb, vb = to_blocks(q), to_blocks(k), to_blocks(v)
    kc, vc = with_prev(kb), with_prev(vb)
    s = jnp.einsum('znqhd,znkhd->znhqk', qb, kc).astype(jnp.float32) / math.sqrt(Dh)
    qi = jnp.arange(W)[:, None]
    kj = jnp.arange(2 * W)[None, :]
    dist = qi + W - kj
    band = (dist >= 0) & (dist <= W)
    valid = (jnp.arange(nb)[:, None, None] > 0) | (kj[None] >= W)
    mask = band[None] & valid
    s = jnp.where(mask[None, :, None], s, NEG)
    m = jnp.max(s, axis=-1, keepdims=True)
    e = jnp.exp(s - m)
    den = jnp.sum(e, axis=-1)
    o = jnp.einsum('znhqk,znkhd->znqhd', e, vc.astype(jnp.float32))
    o = o / jnp.transpose(den, (0, 1, 3, 2))[..., None]
    lse = jnp.transpose(m[..., 0] + jnp.log(den), (0, 1, 3, 2))
    o = o.reshape(B * d, nb * W, H, Dh)[:, :L]
    o = o.reshape(B, d, L, H, Dh).transpose(0, 2, 1, 3, 4).reshape(B, S, H, Dh)
    lse = lse.reshape(B * d, nb * W, H)[:, :L]
    lse = lse.reshape(B, d, L, H).transpose(0, 2, 1, 3).reshape(B, S, H)
    return o, lse


def forgetting_attention(q, k, v, logf):
    B, S, H, Dh = q.shape
    c = jnp.cumsum(logf, axis=1)
    cT = jnp.transpose(c, (0, 2, 1))
    nq = S // Q_BLOCK
    qblk = q.reshape(B, nq, Q_BLOCK, H, Dh).transpose(1, 0, 2, 3, 4)
    cblk = cT.reshape(B, H, nq, Q_BLOCK).transpose(2, 0, 1, 3)
    starts = jnp.arange(nq, dtype=jnp.int32) * Q_BLOCK
    kpos = jnp.arange(S, dtype=jnp.int32)
    vf = v.astype(jnp.float32)
    scale = 1.0 / math.sqrt(Dh)

    def block(args):
        qi, ci, st = args
        s = jnp.einsum('bqhd,bkhd->bhqk', qi, k).astype(jnp.float32) * scale
        s = s + (ci[..., :, None] - cT[:, :, None, :])
        qpos = st + jnp.arange(Q_BLOCK, dtype=jnp.int32)
        s = jnp.where(kpos[None, :] <= qpos[:, None], s, NEG)
        pr = jax.nn.softmax(s, axis=-1)
        return jnp.einsum('bhqk,bkhd->bqhd', pr, vf)

    o = lax.map(block, (qblk, cblk, starts))
    return o.transpose(1, 0, 2, 3, 4).reshape(B, S, H, Dh)


def hierarchical_moe(m, w_rg, b_rg, w_re, b_re, w_up, w_down):
    lg = (jnp.einsum('bsd,dg->bsg', m, w_rg) + b_rg).astype(jnp.float32)
    pg = jax.nn.softmax(lg, axis=-1)
    gidx = jnp.argmax(lg, axis=-1)
    gw = jnp.take_along_axis(pg, gidx[..., None], axis=-1)[..., 0]
    le = (jnp.einsum('bsd,gde->bsge', m, w_re) + b_re).astype(jnp.float32)
    le_sel = jnp.take_along_axis(le, gidx[..., None, None], axis=2)[:, :, 0]
    top_v, top_i = lax.top_k(le_sel, TOP_K)
    tw = jax.nn.softmax(top_v, axis=-1) * gw[..., None]
    eid = gidx[..., None] * EXPERTS_PER_GROUP + top_i
    gate = jnp.sum(jax.nn.one_hot(eid, N_EXPERTS, dtype=jnp.float32) * tw[..., None], axis=-2)
    y = jnp.zeros(m.shape, jnp.float32)
    for e in range(N_EXPERTS):
        hu = jnp.einsum('bsd,df->bsf', m, w_up[e])
        hid = jax.nn.silu(hu[..., :D_EXPERT]) * hu[..., D_EXPERT:]
        y = y + gate[..., e:e + 1] * jnp.einsum('bsf,fd->bsd', hid, w_down[e]).astype(jnp.float32)
    return y.astype(m.dtype)


def setup_inputs(seed: int = 0) -> dict:
    key = jax.random.key(seed)
    ks = jax.random.split(key, 24)
    f32 = jnp.float32

    def nrm(k, shape, fan_in):
        return jax.random.normal(k, shape, f32) * (fan_in ** -0.5)

    def gain(k, shape):
        return 1.0 + 0.1 * jax.random.normal(k, shape, f32)

    x = jax.random.normal(ks[0], (BATCH, SEQ, D_MODEL), f32)
    p = jax.random.normal(ks[1], (DEPTH, BATCH, SEQ, PLE_DIM), f32)
    offset = jax.random.randint(ks[2], (BATCH, 1), 0, 4096, dtype=jnp.int32)
    positions = (offset + jnp.arange(SEQ, dtype=jnp.int32)[None, :]).astype(jnp.int32)
    return {
        "x": x,
        "p": p,
        "positions": positions,
        "g_mix": gain(ks[3], (DEPTH, D_MODEL)),
        "w_in": nrm(ks[4], (DEPTH, D_MODEL, N_IN), D_MODEL),
        "b_f": FORGET_BIAS + 0.1 * jax.random.normal(ks[5], (DEPTH, N_HEADS_B), f32),
        "qn_a": gain(ks[6], (DEPTH, HEAD_DIM)),
        "kn_a": gain(ks[7], (DEPTH, HEAD_DIM)),
        "qn_b": gain(ks[8], (DEPTH, HEAD_DIM)),
        "kn_b": gain(ks[9], (DEPTH, HEAD_DIM)),
        "w_o": nrm(ks[10], (DEPTH, D_MIX, D_MODEL), D_MIX),
        "g_ffn": gain(ks[11], (DEPTH, D_MODEL)),
        "w_rg": nrm(ks[12], (DEPTH, D_MODEL, N_GROUPS), D_MODEL),
        "b_rg": 0.01 * jax.random.normal(ks[13], (DEPTH, N_GROUPS), f32),
        "w_re": nrm(ks[14], (DEPTH, N_GROUPS, D_MODEL, EXPERTS_PER_GROUP), D_MODEL),
        "b_re": 0.01 * jax.random.normal(ks[15], (DEPTH, N_GROUPS, EXPERTS_PER_GROUP), f32),
        "w_up": nrm(ks[16], (DEPTH, N_EXPERTS, D_MODEL, 2 * D_EXPERT), D_MODEL),
        "w_down": nrm(ks[17], (DEPTH, N_EXPERTS, D_EXPERT, D_MODEL), D_EXPERT),
        "g_ple": gain(ks[18], (DEPTH, D_MODEL)),
        "w_ple_gate": nrm(ks[19], (DEPTH, D_MODEL, D_MODEL), D_MODEL),
        "w_ple_proj": nrm(ks[20], (DEPTH, PLE_DIM, D_MODEL), PLE_DIM),
    }


def reference(x, p, positions, g_mix, w_in, b_f, qn_a, kn_a, qn_b, kn_b, w_o,
              g_ffn, w_rg, b_rg, w_re, b_re, w_up, w_down, g_ple, w_ple_gate, w_ple_proj):
    B, S, _ = x.shape
    cos, sin = rope_tables(positions)
    h = x
    for i in range(DEPTH):
        a = rmsnorm(h, g_mix[i])
        proj = jnp.einsum('bsd,dn->bsn', a, w_in[i])
        o0 = 0
        parts = []
        for (width, heads) in ((D_A, N_HEADS_A),) * 3 + ((D_B, N_HEADS_B),) * 3:
            parts.append(proj[..., o0:o0 + width].reshape(B, S, heads, HEAD_DIM))
            o0 += width
        qa, ka, va, qb, kb, vb = parts
        f_logit = proj[..., o0:o0 + N_HEADS_B]

        qa = partial_rope(rmsnorm(qa, qn_a[i]), cos, sin)
        ka = partial_rope(rmsnorm(ka, kn_a[i]), cos, sin)
        outs, lses = [], []
        for (window, dilation) in DILATED_PATTERNS:
            o_p, l_p = dilated_window_attention(qa, ka, va, window, dilation)
            outs.append(o_p)
            lses.append(l_p)
        alpha = jax.nn.softmax(jnp.stack(lses, axis=0), axis=0)
        oa = jnp.sum(alpha[..., None] * jnp.stack(outs, axis=0), axis=0)

        qb = rmsnorm(qb, qn_b[i])
        kb = rmsnorm(kb, kn_b[i])
        logf = jax.nn.log_sigmoid(f_logit.astype(jnp.float32) + b_f[i].astype(jnp.float32))
        ob = forgetting_attention(qb, kb, vb, logf)

        mix = jnp.concatenate([oa, ob], axis=2).reshape(B, S, D_MIX).astype(h.dtype)
        h = h + jnp.einsum('bsm,md->bsd', mix, w_o[i])

        h = h + hierarchical_moe(rmsnorm(h, g_ffn[i]), w_rg[i], b_rg[i], w_re[i], b_re[i],
                                 w_up[i], w_down[i])

        gate = jax.nn.sigmoid(jnp.einsum('bsd,de->bse', rmsnorm(h, g_ple[i]), w_ple_gate[i]).astype(jnp.float32))
        ple = jnp.einsum('bsc,cd->bsd', p[i], w_ple_proj[i]).astype(jnp.float32)
        h = h + (gate * ple).astype(h.dtype)
    return h
```

```python
import math
from contextlib import ExitStack

import numpy as np
import concourse.bass as bass
import concourse.mybir as mybir
from concourse.bass_utils import run_bass_kernel_spmd

F32 = mybir.dt.float32
BF16 = mybir.dt.bfloat16
I32 = mybir.dt.int32
AF = mybir.ActivationFunctionType
ALU = mybir.AluOpType
AX = mybir.AxisListType

ENGS = ("sync", "pe", "act", "dve", "pool")

S = 2048
D = 1024
NT = 16
EPS = 1e-6
N_EXP = 32
BIG = 1.0e30


class Prog:
    def __init__(self, nc, stack, n_dma_sems=8):
        self.nc = nc
        self.streams = {e: [] for e in ENGS}
        self.esem = {}
        for e in ("pe", "act", "dve", "pool"):
            self.esem[e] = stack.enter_context(nc.semaphore("s_" + e))
        self.ecount = {e: 0 for e in ("pe", "act", "dve", "pool")}
        self.dsems, self.dcount, self.drr = {}, {}, {}
        for q, nsem in (("sync", 12), ("act", 8), ("pool", 40)):
            self.dsems[q] = [stack.enter_context(nc.semaphore(f"d_{q}{i}")) for i in range(nsem)]
            self.dcount[q] = [0] * nsem
            self.drr[q] = 0
        self.known = {e: {} for e in ENGS}
        self.last_w = {}
        self.readers = {}
        self.out_tokens = []
        self.n_ops = 0

    def _deps(self, eng, reads, writes):
        deps = []
        for k in reads:
            t = self.last_w.get(k)
            if t is not None:
                deps.append(t)
        for k in writes:
            t = self.last_w.get(k)
            if t is not None:
                deps.append(t)
            deps.extend(self.readers.get(k, ()))
        kn = self.known[eng]
        best = {}
        for (sem, val, seng) in deps:
            if seng == "pe" and eng == "pe":
                continue
            if kn.get(id(sem), 0) >= val:
                continue
            if id(sem) not in best or best[id(sem)][1] < val:
                best[id(sem)] = (sem, val)
        for sem, val in best.values():
            kn[id(sem)] = val
        return list(best.values())

    def _commit(self, token, reads, writes):
        for k in writes:
            self.last_w[k] = token
            self.readers[k] = []
        for k in reads:
            if k in writes:
                continue
            self.readers.setdefault(k, []).append(token)

    def op(self, eng, fn, reads=(), writes=(), inc=True):
        reads, writes = tuple(reads), tuple(writes)
        waits = self._deps(eng, reads, writes)
        if inc:
            self.ecount[eng] += 1
            token = (self.esem[eng], self.ecount[eng], eng)
        else:
            token = (self.esem[eng], self.ecount[eng] + 1, eng)
        self.streams[eng].append((waits, fn, (self.esem[eng], 1) if inc else None))
        self._commit(token, reads, writes)
        self.n_ops += 1
        return token

    def dma(self, q, fn, reads=(), writes=(), is_output=False):
        reads, writes = tuple(reads), tuple(writes)
        waits = self._deps(q, reads, writes)
        j = self.drr[q]
        self.drr[q] = (j + 1) % len(self.dsems[q])
        sem = self.dsems[q][j]
        prev = self.dcount[q][j]
        if prev > 0 and self.known[q].get(id(sem), 0) < prev:
            self.known[q][id(sem)] = prev
            waits = [w for w in waits if w[0] is not sem] + [(sem, prev)]
        self.dcount[q][j] += 16
        token = (sem, self.dcount[q][j], "dma_" + q)
        self.streams[q].append((waits, fn, (sem, 16)))
        self._commit(token, reads, writes)
        if is_output:
            self.out_tokens.append(token)
        self.n_ops += 1
        return token

    def barrier(self):
        toks = []
        for e in ("pe", "act", "dve", "pool"):
            if self.ecount[e] > 0:
                toks.append((self.esem[e], self.ecount[e]))
        for q in ("sync", "act", "pool"):
            for j, s in enumerate(self.dsems[q]):
                if self.dcount[q][j] > 0:
                    toks.append((s, self.dcount[q][j]))
        for e in ENGS:
            waits = []
            for sem, val in toks:
                if self.known[e].get(id(sem), 0) >= val:
                    continue
                self.known[e][id(sem)] = val
                waits.append((sem, val))
            if waits:
                self.streams[e].append((waits, None, None))
        self.last_w = {}
        self.readers = {}

    def finish(self):
        best = {}
        for sem, val, _ in self.out_tokens:
            if id(sem) not in best or best[id(sem)][1] < val:
                best[id(sem)] = (sem, val)
        self.streams["sync"].append((list(best.values()), None, None))

    def emit(self):
        streams = self.streams

        def run(name, e):
            for waits, fn, inc in streams[name]:
                for sem, val in waits:
                    e.wait_ge(sem, val)
                if fn is not None:
                    ins = fn(e)
                    if inc is not None:
                        ins.then_inc(inc[0], inc[1])

        with self.nc.Block() as block:
            @block.sync
            def _(e):
                run("sync", e)

            @block.tensor
            def _(e):
                run("pe", e)

            @block.scalar
            def _(e):
                run("act", e)

            @block.vector
            def _(e):
                run("dve", e)

            @block.gpsimd
            def _(e):
                run("pool", e)


def strided(ap2d, start, step, count):
    return bass.AP(ap2d.tensor, ap2d.offset + start, [list(ap2d.ap[0]), [step, count]])


def bcast_mid(ap2d, reps):
    return bass.AP(ap2d.tensor, ap2d.offset, [list(ap2d.ap[0]), [0, reps], list(ap2d.ap[1])])


def bcast_last(ap2d, reps):
    return bass.AP(ap2d.tensor, ap2d.offset, [list(ap2d.ap[0]), list(ap2d.ap[1]), [0, reps]])


def dram_pbcast(ap1d, parts, n):
    return bass.AP(ap1d.tensor, ap1d.offset, [[0, parts], [1, n]])


class _Stop(Exception):
    pass


class Arena:
    def __init__(self, t, nbytes):
        self.t = t
        self.nbytes = nbytes

    def v(self, off, shape, dtype=BF16, parts=None):
        es = 2 if dtype == BF16 else 4
        n = 1
        for s in shape[1:]:
            n *= s
        nb = n * es
        assert off % 4 == 0 and off + nb <= self.nbytes, (off, nb, self.nbytes)
        p0, p1 = (0, shape[0]) if parts is None else parts
        a = self.t[p0:p1, off // 2:(off + nb) // 2]
        if dtype != BF16:
            a = a.bitcast(dtype)
        if len(shape) == 3:
            a = a.rearrange("p (a b) -> p a b", a=shape[1])
        elif len(shape) == 4:
            a = a.rearrange("p (a b c) -> p a b c", a=shape[1], b=shape[2])
        return a


ARENA_BYTES = 200704
O_XT = 0
O_MIX = 32768
O_R1 = 65536
O_W = 131072
O_TA = 147456
O_TB = 176128

NCM = 128 * 6 + 4 * 32
SPARSE = True
NSLOT = 48
NTILE_RUN = 47
CAP = 256
NSUB = 96


def build_program(debug_stage=None):
    nc = bass.Bass("TRN2", target_bir_lowering=False)
    dr = {}

    def din(name, shape, dt=F32):
        dr[name] = nc.dram_tensor(name, list(shape), dt, kind="ExternalInput").ap()
        return dr[name]

    x = din("x", [S, D])
    p_in = din("p", [S, 256])
    pos = din("pos", [S], I32)
    g_mix = din("g_mix", [D])
    w_in = din("w_in", [D, 3080])
    b_f = din("b_f", [8])
    qn_a = din("qn_a", [64]); kn_a = din("kn_a", [64]); qn_b = din("qn_b", [64]); kn_b = din("kn_b", [64])
    w_o = din("w_o", [D, D])
    g_ffn = din("g_ffn", [D])
    w_r = din("w_r", [D, 36])
    b_r = din("b_r", [36])
    g_ple = din("g_ple", [D])
    w_pg = din("w_pg", [D, D])
    w_pp = din("w_pp", [256, D])
    cmat_d = din("cmat", [128, NCM])
    ident_d = din("ident", [128, 128])
    invf_d = din("invf", [128, 1])
    cvec_d = din("cvec", [128, 96])
    w_up_r = din("w_up_r", [N_EXP * 128 * 4, 2048])
    w_down_r = din("w_down_r", [N_EXP * 128 * 2, 2048])
    out = nc.dram_tensor("out", [S, D], F32, kind="ExternalOutput").ap()
    cps = nc.dram_tensor("cps", [2, 3, 8, S], BF16).ap()
    mrows = nc.dram_tensor("mrows", [S, D], BF16).ap()
    tos = nc.dram_tensor("tos", [NSUB * 128, 16], I32).ap()
    Yd = nc.dram_tensor("Yd", [NSUB * 128, D], F32).ap()

    dbg = {}

    with ExitStack() as st:
        P = Prog(nc, st)
        arena_t = st.enter_context(nc.sbuf_tensor("arena", [128, ARENA_BYTES // 2], BF16))
        A = Arena(arena_t, ARENA_BYTES)
        cmat = st.enter_context(nc.sbuf_tensor("cmat_sb", [128, NCM], BF16))
        ident = st.enter_context(nc.sbuf_tensor("ident_sb", [128, 128], F32))
        smallf = st.enter_context(nc.sbuf_tensor("smallf", [128, 256], F32))
        gate_t = st.enter_context(nc.sbuf_tensor("gate_sb", [128, NT, 32], F32))
        psum = st.enter_context(nc.psum_tensor("psum", [128, 4096], F32))

        def bank(b, n=1):
            return psum[:, b * 512:(b + n) * 512]

        identb = cmat[:, 0:128]
        BD = cmat[:, 128:256]
        ROT = cmat[:, 256:384]
        maskC = cmat[:, 384:512]
        maskP = cmat[:, 512:640]
        mask3 = [cmat[:, 640 + 32 * n: 672 + 32 * n] for n in range(4)]
        LST = cmat[:, 768:896]
        cvec = st.enter_context(nc.sbuf_tensor("cvec_sb", [128, 96], F32))
        M2all = st.enter_context(nc.sbuf_tensor("m2all_sb", [128, NT, 32], F32))
        W12 = st.enter_context(nc.sbuf_tensor("w12_sb", [128, 2, NT], F32))
        S12i = st.enter_context(nc.sbuf_tensor("s12i_sb", [128, 2, NT], I32))
        idxU = st.enter_context(nc.sbuf_tensor("idxu_sb", [128, NSLOT, 4], I32))
        idxD = st.enter_context(nc.sbuf_tensor("idxd_sb", [128, NSLOT, 2], I32))
        TOK = st.enter_context(nc.sbuf_tensor("tok_sb", [128, NSUB], I32))
        tokrep = st.enter_context(nc.sbuf_tensor("tokrep_sb", [128, NT, 16], I32))
        onesm = st.enter_context(nc.sbuf_tensor("onesm_sb", [128, 128], BF16))
        bnd_reg = st.enter_context(nc.gpsimd.register("bnd"))
        P.streams["pool"].append(([], lambda e: e.reg_mov(bnd_reg, N_EXP * 128 * 4 - 1), None))

        invf = smallf[:, 0:1]
        gq_a = smallf[:, 1:2]; gk_a = smallf[:, 2:3]; gq_b = smallf[:, 3:4]; gk_b = smallf[:, 4:5]
        negbf = smallf[0:8, 5:6]
        ss_col = smallf[:, 8:9]
        rs_col = smallf[:, 9:10]
        rt = smallf[:, 16:80]
        bias_r = smallf[:, 96:132]
        L_r = smallf[:, 136:172]
        lem2 = smallf[:, 176:208]
        msk1 = smallf[:, 208:240]

        XT = A.v(O_XT, [128, 8, S])
        MIXT = A.v(O_MIX, [128, 8, S])
        h = A.v(O_R1, [128, NT, D], F32)

        xt = [A.v(O_TA + 4096 * i, [128, D], F32) for i in range(2)]
        sqf = A.v(O_TA + 8192, [128, D], F32)
        hn = [A.v(O_TA + 12288 + 4096 * i, [128, D], F32) for i in range(2)]
        gbc = A.v(O_TA + 20480, [128, D], F32)
        m32 = A.v(O_TA + 24576, [128, 8, 128], F32)

        def mm(o, lhsT, rhs, start, stop, r, w, inc=None):
            P.op("pe", lambda e: e.matmul(o, lhsT, rhs, start=start, stop=stop, skip_group_check=True),
                 r, w, inc=(stop if inc is None else inc))

        def tr32(o, in_, r, w, inc=True):
            P.op("pe", lambda e: e.transpose(o, in_, ident[:, :]), r, w, inc=inc)

        def act(o, in_, func, r, w, bias=None, scale=None):
            kw = {}
            if bias is not None:
                kw["bias"] = bias
            if scale is not None:
                kw["scale"] = scale
            P.op("act", lambda e: e.activation(out=o, in_=in_, func=func, **kw), r, w)

        def tt(eng, o, in0, in1, op, r, w):
            P.op(eng, lambda e: e.tensor_tensor(out=o, in0=in0, in1=in1, op=op), r, w)

        def ts(eng, o, in0, s1, s2, op0, op1, r, w):
            if op1 is None:
                P.op(eng, lambda e: e.tensor_scalar(out=o, in0=in0, scalar1=s1, scalar2=None, op0=op0), r, w)
            else:
                P.op(eng, lambda e: e.tensor_scalar(out=o, in0=in0, scalar1=s1, scalar2=s2, op0=op0, op1=op1), r, w)

        def stt(o, in0, scalar, in1, op0, op1, r, w):
            P.op("dve", lambda e: e.scalar_tensor_tensor(out=o, in0=in0, scalar=scalar, in1=in1, op0=op0, op1=op1), r, w)

        def cp(eng, o, in_, r, w):
            if eng == "act":
                P.op("act", lambda e: e.activation(out=o, in_=in_, func=AF.Copy), r, w)
            else:
                P.op(eng, lambda e: e.tensor_copy(out=o, in_=in_), r, w)

        def red(o, in_, op, r, w):
            P.op("dve", lambda e: e.tensor_reduce(out=o, in_=in_, axis=AX.X, op=op), r, w)

        def recip(o, in_, r, w):
            P.op("dve", lambda e: e.reciprocal(out=o, in_=in_), r, w)

        def memset(eng, ap, val, w):
            P.op(eng, lambda e: e.memset(ap, val), (), w)

        def dma(q, o, in_, r, w, is_output=False):
            P.dma(q, lambda e: e.dma_start(out=o, in_=in_), r, w, is_output=is_output)

        def dbg_out(name, ap_sb, shape, keys, dt=F32):
            d = nc.dram_tensor("dbg_" + name, list(shape), dt, kind="ExternalOutput").ap()
            dbg[name] = d
            dma("sync", d, ap_sb, keys, (), is_output=True)

        def body():
            dma("pool", cmat[:, :], cmat_d, (), ["cmat"])
            dma("sync", ident[:, :], ident_d, (), ["ident"])
            dma("sync", invf, invf_d, (), ["sm_invf"])
            dma("sync", cvec[:, :], cvec_d, (), ["cvec"])
            memset("pool", onesm[:, :], 1.0, ["onesm"])
            for ci, (col, src) in enumerate(((gq_a, qn_a), (gk_a, kn_a), (gq_b, qn_b), (gk_b, kn_b))):
                s2 = src.rearrange("(p o) -> p o", o=1)
                dma("sync", col[0:64, :], s2, (), [("sm_g", ci, 0)])
                dma("sync", col[64:128, :], s2, (), [("sm_g", ci, 1)])
            dma("sync", negbf, b_f.rearrange("(p o) -> p o", o=1), (), ["sm_bf"])
            dma("sync", bias_r, dram_pbcast(b_r, 128, 36), (), ["sm_br"])
            ts("dve", smallf[:, 2:3], smallf[:, 2:3], 0.125, None, ALU.mult, None, [("sm_g", 1, 0), ("sm_g", 1, 1)], ["small"])
            ts("dve", smallf[:, 4:5], smallf[:, 4:5], 0.125, None, ALU.mult, None, [("sm_g", 3, 0), ("sm_g", 3, 1)], ["small"])
            ts("dve", negbf, negbf, -1.0, None, ALU.mult, None, ["sm_bf"], ["small"])
            SMK = ["small", "sm_invf", "sm_br"] + [("sm_g", ci, hf) for ci in range(4) for hf in range(2)]

            def norm_stats(src, src_key, i):
                hb = hn[i]
                hk = ("hn", i)
                act(sqf, src, AF.Square, [src_key], ["sqf"])
                red(ss_col, sqf, ALU.add, ["sqf"], ["ss"])
                act(rs_col, ss_col, AF.Ln, ["ss"], ["rs"], bias=EPS, scale=1.0 / D)
                act(rs_col, rs_col, AF.Exp, ["rs"], ["rs"], scale=-0.5)
                stt(hb, src, rs_col, gbc, ALU.mult, ALU.mult, [src_key, "rs", "gbc"], [hk])

            def norm_tr(t, i, pb0, m32buf=None, m32key=None):
                hb = hn[i]
                hk = ("hn", i)
                pk = [("ps", pb0), ("ps", pb0 + 1)]
                for kc in range(8):
                    tr32(psum[:, pb0 * 512 + kc * 128: pb0 * 512 + (kc + 1) * 128], hb[:, kc * 128:(kc + 1) * 128],
                         [hk, "ident"], [pk[kc // 4]], inc=(kc % 4 == 3))
                pv = bank(pb0, 2).rearrange("p (a b) -> p a b", a=8)
                xk = ("XT", t)
                cp("act", XT[:, 0:4, t * 128:(t + 1) * 128], pv[:, 0:4, :], [pk[0]], [xk])
                cp("dve", XT[:, 4:8, t * 128:(t + 1) * 128], pv[:, 4:8, :], [pk[1]], [xk])
                if m32buf is not None:
                    cp("dve", m32buf[:, 0:4, :], pv[:, 0:4, :], [pk[0]], [m32key])
                    cp("act", m32buf[:, 4:8, :], pv[:, 4:8, :], [pk[1]], [m32key])

            def skew(phases, n):
                for step in range(n + len(phases) - 1):
                    for k_, f_ in enumerate(phases):
                        t_ = step - k_
                        if 0 <= t_ < n:
                            f_(t_)

            dma("sync", gbc, dram_pbcast(g_mix, 128, D), (), ["gbc"])
            def s1A(t):
                i = t % 2
                dma("sync", xt[i], x[t * 128:(t + 1) * 128, :], (), [("xt", i)])
                norm_stats(xt[i], ("xt", i), i)

            def s1B(t):
                norm_tr(t, t % 2, 2 * (t % 2))

            skew([s1A, s1B], NT)
            XTK = [("XT", t) for t in range(NT)]

            if debug_stage == 1:
                dbg_out("XT", XT, [128, 8, S], XTK, BF16)
                raise _Stop()

            COS = A.v(O_R1 + 49152, [128, S])
            SIN = A.v(O_R1 + 53248, [128, S])
            posi = A.v(O_MIX, [128, S], I32)
            ang = A.v(O_MIX + 8192, [128, S], F32)
            kf = A.v(O_MIX + 16384, [128, S], F32)
            ki = A.v(O_MIX + 24576, [128, S], I32)
            dma("sync", posi, dram_pbcast(pos, 128, S), (), ["posi"])
            C1 = 6.28125
            C2 = float(2 * math.pi - 6.28125)

            def sin_table(dst, shift):
                cp("dve", ang, posi, ["posi"], ["ang"])
                if shift == 0.0:
                    ts("dve", ang, ang, invf, None, ALU.mult, None, ["ang", "sm_invf"], ["ang"])
                else:
                    ts("dve", ang, ang, invf, shift, ALU.mult, ALU.add, ["ang", "sm_invf"], ["ang"])
                ts("dve", ki, ang, float(1.0 / (2 * math.pi)), None, ALU.mult, None, ["ang"], ["ki"])
                cp("dve", kf, ki, ["ki"], ["kf"])
                stt(ang, kf, -C1, ang, ALU.mult, ALU.add, ["kf", "ang"], ["ang"])
                stt(ang, kf, -C2, ang, ALU.mult, ALU.add, ["kf", "ang"], ["ang"])
                ts("dve", kf, ang, float(math.pi), float(-2 * math.pi), ALU.is_gt, ALU.mult, ["ang"], ["kf"])
                tt("dve", ang, ang, kf, ALU.add, ["ang", "kf"], ["ang"])
                ts("dve", kf, ang, float(-math.pi), float(2 * math.pi), ALU.is_lt, ALU.mult, ["ang"], ["kf"])
                tt("dve", ang, ang, kf, ALU.add, ["ang", "kf"], ["ang"])
                ts("dve", ang, ang, 3.14159, -3.14159, ALU.min, ALU.max, ["ang"], ["ang"])
                act(dst, ang, AF.Sin, ["ang"], [("tab", shift)])

            sin_table(SIN, 0.0)
            sin_table(COS, float(math.pi / 2))
            TABK = [("tab", 0.0), ("tab", float(math.pi / 2))]

            wf = A.v(O_W + 12288, [128, 8, 8])
            dma("pool", wf, w_in[:, 3072:3080].rearrange("(k p) n -> p k n", p=128), (), ["wf"])
            SP = A.v(O_MIX + 16384, [8, S], F32)
            CS = A.v(O_MIX + 24576, [8, S], F32)
            oc_ = smallf[0:8, 10:11]
            ONES8 = bass.AP(oc_.tensor, oc_.offset, [list(oc_.ap[0]), [0, S]])
            PRT = A.v(O_R1 + 59392, [8, S])
            E1 = A.v(O_R1 + 57344, [8, 512], F32)
            memset("pool", smallf[0:8, 10:11], 1.0, ["ones8"])
            for tg in range(4):
                pk = ("ps", tg % 2)
                for kc in range(8):
                    mm(bank(tg % 2)[0:8, :], wf[:, kc, :], XT[:, kc, tg * 512:(tg + 1) * 512], kc == 0, kc == 7,
                       ["wf"] + XTK[tg * 4:(tg + 1) * 4], [pk])
                act(E1, bank(tg % 2)[0:8, :], AF.Exp, [pk, "small"], ["E1"], bias=negbf, scale=-1.0)
                act(SP[:, tg * 512:(tg + 1) * 512], E1, AF.Ln, ["E1"], ["SP"], bias=1.0, scale=1.0)
            P.op("dve", lambda e: e.tensor_tensor_scan(out=CS, data0=ONES8, data1=SP, initial=0.0, op0=ALU.mult, op1=ALU.add),
                 ["ones8", "SP"], ["CS"])
            R1 = SP
            for j in range(3):
                src = CS if j == 0 else R1
                cp("dve", PRT, src, ["CS", "SP"], ["PRT"])
                dma("sync", cps[0, j, :, :], PRT, ["PRT"], [("cps", 0, j)])
                if j < 2:
                    tt("dve", R1, src, PRT, ALU.subtract, ["CS", "SP", "PRT"], ["SP"])
                ts("dve", PRT, PRT, -1.0, None, ALU.mult, None, ["PRT"], ["PRT"])
                dma("sync", cps[1, j, :, :], PRT, ["PRT"], [("cps", 1, j)])
            if debug_stage == 3:
                dbg_out("CS", CS, [8, S], ["CS"])

            PT = [A.v(O_TB + 1024 * i, [128, 512]) for i in range(4)]
            sqb = [A.v(O_TB + 4096 + 1024 * i, [128, 512]) for i in range(2)]
            lnv = [A.v(O_TB + 6144 + 2048 * i, [128, 512], F32) for i in range(2)]
            rstd = [A.v(O_TB + 10240 + 2048 * i, [128, 512], F32) for i in range(2)]
            qnb = [A.v(O_TB + 14336 + 1024 * i, [128, 512]) for i in range(2)]
            t1b = [A.v(O_TB + 16384 + 2048 * i, [128, 512], F32) for i in range(2)]
            t2b = [A.v(O_TB + 20480 + 2048 * i, [128, 512], F32) for i in range(2)]
            Rb = [A.v(O_TA + 8192 + 2048 * i, [128, 512], F32) for i in range(2)]
            wsl = [A.v(O_W + 6144 * i, [128, 8, 384]) for i in range(2)]

            cnt = {"pt": 0, "set": 0, "sc": 0, "rb": 0}

            def load_wsl(i, cq, ck, cv):
                for j, c0 in enumerate((cq, ck, cv)):
                    src = w_in[:, c0:c0 + 128].rearrange("(k p) n -> p k n", p=128)
                    dma("pool", wsl[i][:, :, j * 128:(j + 1) * 128], src, (), [("wsl", i)])

            def project_T(i, j, tg, pbank):
                pk = ("ps", pbank)
                for kc in range(8):
                    mm(bank(pbank), wsl[i][:, kc, j * 128:(j + 1) * 128], XT[:, kc, tg * 512:(tg + 1) * 512],
                       kc == 0, kc == 7, [("wsl", i)] + XTK[tg * 4:(tg + 1) * 4], [pk])
                return pk

            def qk_norm_rstd(pbank, pk, sbank):
                s = cnt["set"] % 2
                cnt["set"] += 1
                act(sqb[s], bank(pbank), AF.Square, [pk], [("sqb", s)])
                sk = ("ps", sbank)
                mm(bank(sbank), BD, sqb[s], True, True, [("sqb", s), "cmat"], [sk])
                act(lnv[s], bank(sbank), AF.Ln, [sk], [("lnv", s)], bias=EPS, scale=1.0 / 64)
                act(rstd[s], lnv[s], AF.Exp, [("lnv", s)], [("rstd", s)], scale=-0.5)
                return s

            def vaug_build(VTb, vtk, VA, vak, tiles, pb_list):
                nb = len(tiles) // 4
                for b4 in range(nb):
                    pb = pb_list[b4 % len(pb_list)]
                    pk = ("ps", pb)
                    for tt_ in range(4):
                        mm(psum[:, pb * 512 + tt_ * 128: pb * 512 + (tt_ + 1) * 128], tiles[b4 * 4 + tt_], identb,
                           True, True, [vtk, "cmat"], [pk], inc=(tt_ == 3))
                    src = bank(pb).rearrange("p (t h d) -> p t h d", t=4, h=2)
                    eng = "dve" if b4 % 2 == 0 else "act"
                    cp(eng, VA[:, b4 * 4:(b4 + 1) * 4, :, 0:64], src, [pk], [vak])

            VA1 = A.v(O_R1, [128, NT, 2, 128])
            VA4 = A.v(O_R1 + 8192, [128, NT, 2, 128])
            VA16 = A.v(O_R1 + 16384, [128, NT, 2, 128])
            QTb = [A.v(O_R1 + 24576 + 4096 * i, [128, S]) for i in range(2)]
            KTb = [A.v(O_R1 + 32768 + 4096 * i, [128, S]) for i in range(2)]
            VTb = [A.v(O_R1 + 40960 + 4096 * i, [128, S]) for i in range(2)]
            for VA in (VA1, VA4, VA16):
                memset("pool", VA, 1.0, ["VA1", "VA4", "VA16"])

            def attn_norm_out(O_bank, ok, hp_chunk, hh, n):
                r = cnt["rb"] % 2
                cnt["rb"] += 1
                act(Rb[r][0:64, :], bank(O_bank)[64:128, :], AF.Ln, [ok], [("Rb", r)])
                act(Rb[r][0:64, :], Rb[r][0:64, :], AF.Exp, [("Rb", r)], [("Rb", r)], scale=-1.0)
                tt("dve", MIXT[64 * hh:64 * hh + 64, hp_chunk, n * 512:(n + 1) * 512], bank(O_bank)[0:64, :], Rb[r][0:64, :],
                   ALU.mult, [ok, ("Rb", r)], [("MIXT", hp_chunk, n)])

            def exp_mask(sbank, c0, c1, mask_ap3, mcols, eng):
                pi = cnt["pt"] % 4
                cnt["pt"] += 1
                pk = ("PT", pi)
                act(PT[pi][:, c0:c1], bank(sbank)[:, c0:c1], AF.Exp, [("ps", sbank)], [pk])
                if mask_ap3 is not None:
                    m0, m1 = mcols
                    view = PT[pi][:, m0:m1]
                    if len(mask_ap3.shape) == 3:
                        view = view.rearrange("p (a b) -> p a b", a=mask_ap3.shape[1])
                    tt(eng, view, view, mask_ap3, ALU.mult, [pk, "cmat"], [pk])
                return PT[pi], pk

            NSC = 4
            pending_norm = []
            NORM_DEFER = 3

            def run_pipeline(steps, LA=3):
                nst = len(steps)
                for s_ in range(nst + LA):
                    if s_ < nst:
                        qk_fn(steps[s_])
                    k_ = s_ - LA
                    if k_ >= 0:
                        ex_fn(steps[k_])
                        pv_fn(steps[k_])
                    for pn in list(pending_norm):
                        pn[0] -= 1
                        if pn[0] <= 0:
                            u_ = pn[1]
                            attn_norm_out(u_["Ob"], u_["ok"], u_["chunk"], u_["hh"], u_["n"])
                            pending_norm.remove(pn)
                for pn in list(pending_norm):
                    u_ = pn[1]
                    attn_norm_out(u_["Ob"], u_["ok"], u_["chunk"], u_["hh"], u_["n"])
                    pending_norm.remove(pn)

            def qk_fn(stp):
                sb_ = cnt["sc"] % NSC
                cnt["sc"] += 1
                stp["sb"] = sb_
                sk = ("ps", sb_)
                L = stp["qk_list"]
                for idx, (c0, c1, lhsT, rhs) in enumerate(L):
                    mm(bank(sb_)[:, c0:c1], lhsT, rhs, True, True, stp["qk_keys"], [sk], inc=(idx == len(L) - 1))

            def ex_fn(stp):
                stp["pt"], stp["ptk"] = exp_mask(stp["sb"], stp["c0"], stp["c1"], stp["mask"], stp["mcols"], stp["meng"])

            def pv_fn(stp):
                u = stp["u"]
                L = stp["pv_list"]
                for idx, (o_ap, lhsT, vak, p0, p1) in enumerate(L):
                    last = stp["last"] and idx == len(L) - 1
                    mm(o_ap, lhsT, stp["pt"][:, p0:p1], u["first"], last, [stp["ptk"], vak], [u["ok"]], inc=True)
                    u["first"] = False
                if stp["last"]:
                    pending_norm.append([NORM_DEFER, u])

            def new_unit(chunk, hh, n):
                uidx = cnt["unit"]
                cnt["unit"] += 1
                Ob = 4 + uidx % 4
                return dict(Ob=Ob, ok=("ps", Ob), first=True, hh=hh, n=n, chunk=chunk)

            def qk_chains(i, chains, rope):
                nch = len(chains)
                st_ = [dict() for _ in range(nch)]

                def phA(c):
                    j, tg, dst, dk, gain = chains[c]
                    pb = c % 3
                    pk = project_T(i, j, tg, pb)
                    s = c % 2
                    act(sqb[s], bank(pb), AF.Square, [pk], [("sqb", s)])
                    st_[c].update(pb=pb, pk=pk, s=s)

                def phB(c):
                    j, tg, dst, dk, gain = chains[c]
                    pb, pk, s = st_[c]["pb"], st_[c]["pk"], st_[c]["s"]
                    sbank = 3 + c % 2
                    sk = ("ps", sbank)
                    mm(bank(sbank), BD, sqb[s], True, True, [("sqb", s), "cmat"], [sk])
                    act(lnv[s], bank(sbank), AF.Ln, [sk], [("lnv", s)], bias=EPS, scale=1.0 / 64)
                    act(rstd[s], lnv[s], AF.Exp, [("lnv", s)], [("rstd", s)], scale=-0.5)
                    if rope:
                        stt(qnb[s], bank(pb), gain, rstd[s], ALU.mult, ALU.mult, [pk, ("rstd", s)] + SMK, [("qnb", s)])
                    else:
                        cs_ = slice(tg * 512, (tg + 1) * 512)
                        stt(dst[0:64, 0, cs_], bank(pb)[0:64, :], gain[0:64, :], rstd[s][0:64, :],
                            ALU.mult, ALU.mult, [pk, ("rstd", s)] + SMK, [dk])
                        stt(dst[0:64, 1, cs_], bank(pb)[64:128, :], gain[64:128, :], rstd[s][64:128, :],
                            ALU.mult, ALU.mult, [pk, ("rstd", s)] + SMK, [dk])

                def phC(c):
                    j, tg, dst, dk, gain = chains[c]
                    s = st_[c]["s"]
                    rb = 5 + c % 2
                    rk = ("ps", rb)
                    cs_ = slice(tg * 512, (tg + 1) * 512)
                    mm(bank(rb), ROT, qnb[s], True, True, [("qnb", s), "cmat"], [rk])
                    tt("pool", t1b[s], qnb[s], COS[:, cs_], ALU.mult, [("qnb", s), TABK[1]], [("t1", s)])
                    tt("dve", t2b[s], bank(rb), SIN[:, cs_], ALU.mult, [rk, TABK[0]], [("t2", s)])
                    tt("pool", dst[:, cs_], t1b[s], t2b[s], ALU.add, [("t1", s), ("t2", s)], [dk])

                for step in range(nch + 2):
                    if step < nch:
                        phA(step)
                    if 0 <= step - 1 < nch:
                        phB(step - 1)
                    if rope and 0 <= step - 2 < nch:
                        phC(step - 2)

            def v_proj(i, VT, vk_):
                for tg in range(4):
                    pb = tg % 3
                    pk = project_T(i, 2, tg, pb)
                    cp("act" if tg % 2 == 0 else "dve", VT[:, tg * 512:(tg + 1) * 512], bank(pb), [pk], [vk_])

            cnt["unit"] = 0
            load_wsl(0, 0, 512, 1024)
            for hp in range(4):
                i = hp % 2
                QT, KT, VT = QTb[i], KTb[i], VTb[i]
                qk_, kk_, vk_ = ("QT", i), ("KT", i), ("VT", i)
                chains = [(j, tg, dst, dk, gain) for j, (dst, dk, gain) in enumerate(((QT, qk_, gq_a), (KT, kk_, gk_a)))
                          for tg in range(4)]
                qk_chains(i, chains, True)
                v_proj(i, VT, vk_)
                vaug_build(VT, vk_, VA1, "VA1", [VT[:, 128 * t:128 * (t + 1)] for t in range(NT)], [3, 4, 6, 7])
                vaug_build(VT, vk_, VA4, "VA4", [strided(VT, 512 * (t // 4) + (t % 4), 4, 128) for t in range(NT)], [3, 4, 6, 7])
                vaug_build(VT, vk_, VA16, "VA16", [strided(VT, t, 16, 128) for t in range(NT)], [3, 4, 6, 7])
                if hp < 3:
                    load_wsl((hp + 1) % 2, 128 * (hp + 1), 512 + 128 * (hp + 1), 1024 + 128 * (hp + 1))
                else:
                    load_wsl(0, 1536, 2048, 2560)

                if debug_stage == 2 and hp == 0:
                    dbg_out("QT", QT, [128, S], [qk_], BF16)
                    dbg_out("KT", KT, [128, S], [kk_], BF16)
                    dbg_out("VT", VT, [128, S], [vk_], BF16)
                    dbg_out("VA4", VA4, [128, NT, 2, 128], ["VA4"], BF16)
                    dbg_out("COS", COS, [128, S], TABK, BF16)
                    dbg_out("SIN", SIN, [128, S], TABK, BF16)

                steps = []
                for n in range(4):
                    for hh in range(2):
                        u = new_unit(hp, hh, n)
                        Ob = u["Ob"]
                        Kh = KT[64 * hh:64 * hh + 64, :]
                        Qh = QT[64 * hh:64 * hh + 64, :]
                        qkk = [kk_, qk_]
                        steps.append(dict(u=u, qk_keys=qkk, c0=0, c1=512, mask=bcast_mid(maskC, 4), mcols=(0, 512), meng="pool", last=False,
                                          qk_list=[(128 * j, 128 * (j + 1), Kh[:, 128 * (4 * n + j):128 * (4 * n + j + 1)],
                                                    Qh[:, 128 * (4 * n + j):128 * (4 * n + j + 1)]) for j in range(4)],
                                          pv_list=[(bank(Ob)[:, 128 * j:128 * (j + 1)], VA1[:, 4 * n + j, hh, :], "VA1", 128 * j, 128 * (j + 1))
                                                   for j in range(4)]))
                        j0 = 1 if n == 0 else 0
                        steps.append(dict(u=u, qk_keys=qkk, c0=128 * j0, c1=512, mask=bcast_mid(maskP, 4 - j0), mcols=(128 * j0, 512), meng="dve", last=False,
                                          qk_list=[(128 * j, 128 * (j + 1), Kh[:, 128 * (4 * n + j - 1):128 * (4 * n + j)],
                                                    Qh[:, 128 * (4 * n + j):128 * (4 * n + j + 1)]) for j in range(j0, 4)],
                                          pv_list=[(bank(Ob)[:, 128 * j:128 * (j + 1)], VA1[:, 4 * n + j - 1, hh, :], "VA1", 128 * j, 128 * (j + 1))
                                                   for j in range(j0, 4)]))
                        for prev in (0, 1):
                            if prev and n == 0:
                                continue
                            steps.append(dict(u=u, qk_keys=qkk, c0=0, c1=512, mask=bcast_mid(maskP if prev else maskC, 4), mcols=(0, 512),
                                              meng="dve", last=False,
                                              qk_list=[(128 * r4, 128 * (r4 + 1), strided(Kh, 512 * (n - prev) + r4, 4, 128),
                                                        strided(Qh, 512 * n + r4, 4, 128)) for r4 in range(4)],
                                              pv_list=[(strided(bank(Ob), r4, 4, 128), VA4[:, 4 * (n - prev) + r4, hh, :], "VA4", 128 * r4, 128 * (r4 + 1))
                                                       for r4 in range(4)]))
                        steps.append(dict(u=u, qk_keys=qkk, c0=0, c1=512, mask=bcast_mid(mask3[n], 16), mcols=(0, 512), meng="dve", last=True,
                                          qk_list=[(32 * r, 32 * (r + 1), strided(Kh, r, 16, 128), strided(Qh, 512 * n + r, 16, 32)) for r in range(16)],
                                          pv_list=[(strided(bank(Ob), r, 16, 32), VA16[:, r, hh, :], "VA16", 32 * r, 32 * (r + 1)) for r in range(16)]))
                run_pipeline(steps)

            if debug_stage == 2:
                dbg_out("MIXA", MIXT[:, 0:4, :], [128, 4, S], [("MIXT", c, n) for c in range(4) for n in range(4)], BF16)
                raise _Stop()

            P.barrier()
            QP = [A.v(O_R1 + 8192 * i, [128, 2, S]) for i in range(2)]
            KP = [A.v(O_R1 + 16384 + 8192 * i, [128, 2, S]) for i in range(2)]
            VTB = [A.v(O_R1 + 32768 + 4096 * i, [128, S]) for i in range(2)]
            VAB = [A.v(O_R1 + 40960 + 8192 * i, [128, NT, 2, 128]) for i in range(2)]
            for i in range(2):
                memset("pool", VAB[i], 1.0, [("VAB", i)])
                memset("pool", QP[i][64:70, :, :], 1.0, [("QP", i)])
                memset("pool", KP[i][64:70, :, :], 1.0, [("KP", i)])

            for hp in range(4):
                i = hp % 2
                VT = VTB[i]
                vk_ = ("VTB", i)
                chains = [(j, tg, dst, dk, gain) for j, (dst, dk, gain) in enumerate(((QP[i], ("QP", i), gq_b), (KP[i], ("KP", i), gk_b)))
                          for tg in range(4)]
                qk_chains(i, chains, False)
                dma("sync", QP[i][64:67, :, :], cps[1, :, 2 * hp:2 * hp + 2, :], [("cps", 1, j_) for j_ in range(3)], [("QP", i)])
                dma("sync", KP[i][67:70, :, :], cps[0, :, 2 * hp:2 * hp + 2, :], [("cps", 0, j_) for j_ in range(3)], [("KP", i)])
                v_proj(i, VT, vk_)
                vaug_build(VT, vk_, VAB[i], ("VAB", i), [VT[:, 128 * t:128 * (t + 1)] for t in range(NT)], [3, 4, 6, 7])
                if hp < 3:
                    load_wsl((hp + 1) % 2, 1536 + 128 * (hp + 1), 2048 + 128 * (hp + 1), 2560 + 128 * (hp + 1))
                if debug_stage == 3 and hp == 0:
                    dbg_out("QP", QP[i][0:70, :, :], [70, 2, S], [("QP", i)], BF16)
                    dbg_out("KP", KP[i][0:70, :, :], [70, 2, S], [("KP", i)], BF16)

                steps = []
                for n in range(4):
                    for hh in range(2):
                        u = new_unit(4 + hp, hh, n)
                        Ob = u["Ob"]
                        Kh = KP[i][0:70, hh, :]
                        Qh = QP[i][0:70, hh, :]
                        for jb in range(4 * n + 4):
                            jj = jb - 4 * n
                            c0 = 0 if jj < 0 else 128 * jj
                            steps.append(dict(u=u, qk_keys=[("KP", i), ("QP", i)], c0=c0, c1=512,
                                              mask=(None if jj < 0 else maskC), mcols=(c0, c0 + 128), meng="dve",
                                              last=(jb == 4 * n + 3),
                                              qk_list=[(c0, 512, Kh[:, 128 * jb:128 * (jb + 1)], Qh[:, 512 * n + c0:512 * (n + 1)])],
                                              pv_list=[(bank(Ob)[:, c0:512], VAB[i][:, jb, hh, :], ("VAB", i), c0, 512)]))
                run_pipeline(steps)

            MIXK = [("MIXT", c, n) for c in range(8) for n in range(4)]
            if debug_stage == 3:
                dbg_out("MIXT", MIXT, [128, 8, S], MIXK, BF16)
                raise _Stop()

            P.barrier()
            WO = A.v(O_W, [128, 8, D])
            dma("pool", WO, w_o.rearrange("(k p) n -> p k n", p=128), (), ["WO"])
            WR = A.v(O_TB, [128, 8, 36], F32)
            dma("sync", WR, w_r.rearrange("(k p) n -> p k n", p=128), (), ["WR"])
            dma("sync", gbc, dram_pbcast(g_ffn, 128, D), (), ["gbc"])
            M1all = A.v(O_TB + 2048, [128, NT, 32], F32)
            gw = rt[:, 0:1]; gmax = rt[:, 1:2]; ngmax = rt[:, 2:3]; sg = rt[:, 3:4]
            m1 = rt[:, 4:5]; m2 = rt[:, 5:6]; dd = rt[:, 6:7]; w1 = rt[:, 7:8]; w2 = rt[:, 8:9]
            ohg = rt[:, 12:16]; eg = rt[:, 16:20]; pen = rt[:, 20:24]
            g1 = rt[:, 24:56]
            RK = ["rt"]
            m32b = [m32, A.v(O_TB + 20480, [128, 8, 128], F32)]

            def s3A(t):
                i = t % 2
                dma("sync", xt[i], x[t * 128:(t + 1) * 128, :], (), [("xt", i)])
                for half in range(2):
                    pk = ("ps", half)
                    for c in range(8):
                        mm(bank(half), MIXT[:, c, t * 128:(t + 1) * 128], WO[:, c, half * 512:(half + 1) * 512], c == 0, c == 7,
                           ["WO"] + [("MIXT", c, t // 4)], [pk])
                    tt("dve", h[:, t, half * 512:(half + 1) * 512], bank(half), xt[i][:, half * 512:(half + 1) * 512], ALU.add,
                       [pk, ("xt", i)], [("h", t)])

            def s3B(t):
                norm_stats(h[:, t, :], ("h", t), t % 2)

            def s3C(t):
                i = t % 2
                norm_tr(t, i, 2 + 2 * i, m32b[i], ("m32", i))
                if SPARSE:
                    dma("pool", mrows[t * 128:(t + 1) * 128, :], hn[i], [("hn", i)], [("mrows", t)])

            def s3D(t):
                rk = ("ps", 6)
                for kc in range(8):
                    mm(bank(6)[:, 0:36], m32b[t % 2][:, kc, :], WR[:, kc, :], kc == 0, kc == 7, [("m32", t % 2), "WR"], [rk])
                tt("dve", L_r, bank(6)[:, 0:36], bias_r, ALU.add, [rk, "sm_br"], ["L"])
                red(gmax, L_r[:, 0:4], ALU.max, ["L"], RK)
                ts("dve", ohg, L_r[:, 0:4], gmax, None, ALU.is_equal, None, ["L"] + RK, RK)
                ts("dve", ngmax, gmax, -1.0, None, ALU.mult, None, RK, RK)
                act(eg, L_r[:, 0:4], AF.Exp, ["L"] + RK, RK, bias=ngmax, scale=1.0)
                red(sg, eg, ALU.add, RK, RK)
                recip(gw, sg, RK, RK)
                ts("dve", pen, ohg, -1.0, BIG, ALU.add, ALU.mult, RK, RK)
                lem = L_r[:, 4:36]
                tt("dve", lem.rearrange("p (g e) -> p g e", g=4), lem.rearrange("p (g e) -> p g e", g=4), bcast_last(pen, 8),
                   ALU.add, ["L"] + RK, ["L"])
                red(m1, lem, ALU.max, ["L"], RK)
                ts("dve", msk1, lem, m1, None, ALU.is_equal, None, ["L"] + RK, ["msk1"])
                stt(lem2, msk1, -BIG, lem, ALU.mult, ALU.add, ["msk1", "L"], ["lem2"])
                red(m2, lem2, ALU.max, ["lem2"], RK)
                ts("dve", lem2, lem2, m2, None, ALU.is_equal, None, ["lem2"] + RK, ["lem2"])
                tt("dve", dd, m2, m1, ALU.subtract, RK, RK)
                act(dd, dd, AF.Exp, RK, RK)
                ts("dve", dd, dd, 1.0, None, ALU.add, None, RK, RK)
                recip(w1, dd, RK, RK)
                tt("dve", w1, w1, gw, ALU.mult, RK, RK)
                tt("dve", w2, gw, w1, ALU.subtract, RK, RK)
                ts("dve", g1, msk1, w1, None, ALU.mult, None, ["msk1"] + RK, RK)
                stt(gate_t[:, t, :], lem2, w2, g1, ALU.mult, ALU.add, ["lem2"] + RK, [("gate", t)])
                if SPARSE:
                    cp("pool", M2all[:, t, :], lem2, ["lem2"], [("M2", t)])
                    cp("pool", M1all[:, t, :], msk1, ["msk1"], [("M1", t)])
                    cp("dve", W12[:, 0, t:t + 1], w1, RK, [("W12", t)])
                    cp("dve", W12[:, 1, t:t + 1], w2, RK, [("W12", t)])


            skew([s3A, s3B, s3C, s3D], NT)

            HK = [("h", t) for t in range(NT)]
            GK = [("gate", t) for t in range(NT)]
            if debug_stage == 4:
                dbg_out("h1", h, [128, NT, D], HK)
                dbg_out("gate", gate_t[:, :, :], [128, NT, 32], GK)
                dbg_out("XT2", XT, [128, 8, S], XTK, BF16)
                raise _Stop()

            if SPARSE:
                pidx = cvec[:, 0:1]
                svals = cvec[:, 1:65]
                TS = A.v(O_TB + 4096, [128, NT, 32], F32)
                BASE = A.v(O_TB + 6144, [128, NT, 32], F32)
                SLF = A.v(O_TB + 8192, [128, NT, 32], F32)
                TMPS = A.v(O_TB + 10240, [128, NT, 32], F32)
                MB = A.v(O_TB + 12288, [128, NT * 32])
                CMP = A.v(O_TB + 14336, [128, NSLOT, 32], F32)
                CT = rt
                M1K = [("M1", t) for t in range(NT)]
                M2K = [("M2", t) for t in range(NT)]
                tt("dve", MB.rearrange("p (a b) -> p a b", a=NT), M1all, M2all[:, :, :], ALU.add, M1K + M2K, ["MB"])
                mm(bank(0), onesm[:, :], MB, True, True, ["MB", "onesm"], [("ps", 0)])
                mm(bank(1), LST, MB, True, True, ["MB", "cmat"], [("ps", 1)])
                cp("act", TS, bank(0).rearrange("p (a b) -> p a b", a=NT), [("ps", 0)], ["TS"])
                memset("pool", BASE[:, 0, :], 0.0, ["BASE"])
                for j in range(1, NT):
                    tt("dve", BASE[:, j, :], BASE[:, j - 1, :], TS[:, j - 1, :], ALU.add, ["BASE", "TS"], ["BASE"])
                cnt_e = CT[:, 0:32]
                yv = smallf[:, 176:208]
                kfv = smallf[:, 208:240]
                kiv = A.v(O_TB + 1152, [128, 32], I32)
                tt("dve", cnt_e, BASE[:, NT - 1, :], TS[:, NT - 1, :], ALU.add, ["BASE", "TS"], ["cnt"])
                ts("dve", yv, cnt_e, float(CAP - 1), 1.0 / CAP, ALU.add, ALU.mult, ["cnt"], ["yv"])
                cp("dve", kiv, yv, ["yv"], ["kiv"])
                cp("dve", kfv, kiv, ["kiv"], ["kfv"])
                tt("dve", cnt_e, kfv, yv, ALU.is_gt, ["kfv", "yv"], ["cnt"])
                tt("dve", kfv, kfv, cnt_e, ALU.subtract, ["kfv", "cnt"], ["kfv"])
                oc2 = smallf[:, 10:11]
                ones32 = bass.AP(oc2.tensor, oc2.offset, [list(oc2.ap[0]), [0, 32]])
                memset("pool", smallf[:, 10:11], 1.0, ["ones8"])
                cum = CT[:, 32:64]
                P.op("dve", lambda e: e.tensor_tensor_scan(out=cum, data0=ones32, data1=kfv, initial=0.0, op0=ALU.mult, op1=ALU.add),
                     ["ones8", "kfv"], ["cum"])
                offv = yv
                tt("dve", offv, cum, kfv, ALU.subtract, ["cum", "kfv"], ["yv"])
                ts("dve", offv, offv, float(CAP), None, ALU.mult, None, ["yv"], ["yv"])
                tt("dve", SLF, bank(1).rearrange("p (a b) -> p a b", a=NT), BASE, ALU.add, [("ps", 1), "BASE"], ["SLF"])
                tt("dve", SLF, SLF, bcast_mid(offv, NT), ALU.add, ["SLF", "yv"], ["SLF"])
                S12f = CT[:, 0:32].rearrange("p (a b) -> p a b", a=2)
                tt("dve", TMPS, SLF, M1all, ALU.mult, ["SLF"] + M1K, ["TMPS"])
                red(S12f[:, 0, :], TMPS, ALU.add, ["TMPS"], ["S12f"])
                tt("dve", TMPS, SLF, M2all[:, :, :], ALU.mult, ["SLF"] + M2K, ["TMPS"])
                red(S12f[:, 1, :], TMPS, ALU.add, ["TMPS"], ["S12f"])
                cp("dve", S12i[:, :, :], S12f, ["S12f"], ["S12i"])
                tt("dve", CMP, bcast_mid(cum, NSLOT), bcast_last(svals[:, 0:NSLOT], 32), ALU.is_le, ["cum", "cvec"], ["CMP"])
                es = A.v(O_TB + 1280, [128, NSLOT], F32)
                red(es, CMP, ALU.add, ["CMP"], ["es"])
                ts("dve", es, es, 128.0, None, ALU.mult, None, ["es"], ["es"])
                ts("dve", es, es, pidx, None, ALU.add, None, ["es", "cvec"], ["es"])
                for q in range(4):
                    ts("dve", idxU[:, :, q], es, 4.0, float(q), ALU.mult, ALU.add, ["es"], [("idxU", q)])
                bigf = A.v(O_TB + 1792, [128, NSLOT], F32)
                ts("dve", bigf, es, float(N_EXP * 128), 20000.0, ALU.is_ge, ALU.mult, ["es"], ["bigf"])
                for q in range(2):
                    stt(idxD[:, :, q], es, 2.0, bigf, ALU.mult, ALU.add, ["es", "bigf"], [("idxD", q)])
                    if q:
                        ts("dve", idxD[:, :, q], idxD[:, :, q], 1, None, ALU.add, None, [("idxD", q)], [("idxD", q)])
                IDXK = [("idxU", q) for q in range(4)] + [("idxD", q) for q in range(2)]
                ZT = A.v(O_TA, [128, NSUB * 16], I32)
                memset("pool", ZT, 0, ["ZT"])
                dma("sync", tos.rearrange("(p a) b -> p (a b)", p=128), ZT, ["ZT"], ["tos"])
                tokf = A.v(O_TB + 1536, [128, NT], F32)
                ts("dve", tokf, cvec[:, 65:81], pidx, None, ALU.add, None, ["cvec"], ["tokf"])
                cp("dve", tokrep[:, :, :], bcast_last(tokf, 16), ["tokf"], ["tokrep"])
                for j in range(NT):
                    for k in range(2):
                        P.dma("pool", lambda e, j=j, k=k: e.indirect_dma_start(
                            out=tos, out_offset=bass.IndirectOffsetOnAxis(ap=S12i[:, k, j:j + 1], axis=0),
                            in_=tokrep[:, j, :], in_offset=None), ["S12i", "tokrep", "tos"], [("tosw", j, k)])
                TOSK = [("tosw", j, k) for j in range(NT) for k in range(2)]
                if debug_stage == 6:
                    dbg_out("S12", S12i[:, :, :], [128, 2, NT], ["S12i"], I32)
                    dbg_out("idxU", idxU[:, :, :], [128, NSLOT, 4], IDXK, I32)
                    dbg_out("cum", cum, [128, 32], ["cum"])
                    TOKD = A.v(O_TA + 8192, [128, NSUB, 16], I32)
                    dma("sync", TOKD, tos.rearrange("(s q) r -> q s r", q=128), TOSK, ["TOKD"])
                    dbg_out("tos", TOKD, [128, NSUB, 16], ["TOKD"], I32)
                    raise _Stop()

                P.barrier()
                NB = 3
                WU = [A.v(O_MIX, [128, 8, 1024]), A.v(O_MIX + 16384, [128, 8, 1024]), A.v(O_XT, [128, 8, 1024])]
                WD = [A.v(O_W, [128, 4, D]), A.v(O_W + 8192, [128, 4, D]), A.v(O_XT + 16384, [128, 4, D])]
                XGa = [O_TA, O_TA + 4096, O_XT + 24576]
                XG = [[A.v(XGa[i] + 2048 * u, [128, D]) for u in range(2)] for i in range(NB)]
                XGT = [A.v(O_TA + 8192 + 4096 * i, [128, 8, 256]) for i in range(2)]
                HIDs = [A.v(O_TB + 2048 * i, [128, 4, 256]) for i in range(2)]
                SAs = [A.v(O_TB + 4096 + 2048 * i, [128, 1024]) for i in range(2)]
                YS = [A.v(O_TB + 8192 + 4096 * i, [128, D], F32) for i in range(2)]
                n_slot_run = NTILE_RUN if debug_stage != 7 else 4
                ysc = [0]
                P.dma("sync", lambda e: e.dma_start(out=TOK[:, :], in_=tos.rearrange("(s q) r -> q s r", q=128)[:, :, 0],
                                                    allow_slow_non_contiguous=True), (), ["TOK"])

                lo_, hi_ = 0, n_slot_run - 1
                order = []
                while lo_ <= hi_:
                    order.append(lo_)
                    lo_ += 1
                    if lo_ <= hi_ and len(order) % 2 == 1 and hi_ >= 32:
                        order.append(hi_)
                        hi_ -= 1

                def bufi(k):
                    return (k // 2) % 2 if k % 2 == 0 else 2

                def slot_load(k):
                    i = bufi(k)
                    sl = order[k]
                    for u in range(2):
                        ss = 2 * sl + u
                        P.dma("pool", lambda e, ss=ss, u=u: e.indirect_dma_start(
                            out=XG[i][u], out_offset=None, in_=mrows, in_offset=bass.IndirectOffsetOnAxis(ap=TOK[:, ss:ss + 1], axis=0)),
                            ["TOK"], [("XG", i, u)])
                    for q in range(4):
                        P.dma("pool", lambda e, q=q: e.indirect_dma_start(
                            out=WU[i][:, 2 * q:2 * q + 2, :].rearrange("p a b -> p (a b)"), out_offset=None, in_=w_up_r,
                            in_offset=bass.IndirectOffsetOnAxis(ap=idxU[:, sl, q:q + 1], axis=0),
                            bounds_check=bnd_reg, oob_is_err=False), (), [("WU", i, q)])
                    for q in range(2):
                        P.dma("pool", lambda e, q=q: e.indirect_dma_start(
                            out=WD[i][:, 2 * q:2 * q + 2, :].rearrange("p a b -> p (a b)"), out_offset=None, in_=w_down_r,
                            in_offset=bass.IndirectOffsetOnAxis(ap=idxD[:, sl, q:q + 1], axis=0),
                            bounds_check=bnd_reg, oob_is_err=False), (), [("WD", i, q)])

                def tile_T(k):
                    i = bufi(k)
                    j2 = k % 2
                    for u in range(2):
                        for kc in range(8):
                            mm(psum[:, kc * 128:(kc + 1) * 128], XG[i][u][:, kc * 128:(kc + 1) * 128], identb, True, True,
                               [("XG", i, u), "cmat"], [("ps", kc // 4)], inc=(kc % 4 == 3))
                        pv_ = bank(0, 2).rearrange("p (a b) -> p a b", a=8)
                        cp("act", XGT[j2][:, 0:4, u * 128:(u + 1) * 128], pv_[:, 0:4, :], [("ps", 0)], [("XGT", j2)])
                        cp("dve", XGT[j2][:, 4:8, u * 128:(u + 1) * 128], pv_[:, 4:8, :], [("ps", 1)], [("XGT", j2)])

                def tile_U(k):
                    i = bufi(k)
                    j2 = k % 2
                    for part in range(2):
                        for fc in range(4):
                            bnk = 2 + 2 * part + fc // 2
                            pk = ("ps", bnk)
                            fcol = fc + 4 * part
                            c0 = (fc % 2) * 256
                            for kc in range(8):
                                mm(bank(bnk)[:, c0:c0 + 256], WU[i][:, kc, fcol * 128:(fcol + 1) * 128], XGT[j2][:, kc, :],
                                   kc == 0, kc == 7, [("WU", i, kc // 2), ("XGT", j2)], [pk], inc=(kc == 7 and fc % 2 == 1))
                    act(SAs[j2], bank(2, 2), AF.Silu, [("ps", 2), ("ps", 3)], [("SAs", j2)])
                    tt("dve", HIDs[j2].rearrange("p a b -> p (a b)"), SAs[j2], bank(4, 2), ALU.mult, [("SAs", j2), ("ps", 4), ("ps", 5)], [("HIDs", j2)])

                def tile_D(k):
                    i = bufi(k)
                    j2 = k % 2
                    sl = order[k]
                    for u in range(2):
                        yi = ysc[0] % 2
                        ysc[0] += 1
                        for half in range(2):
                            pk = ("ps", 6 + half)
                            for fc in range(4):
                                mm(bank(6 + half), HIDs[j2][:, fc, u * 128:(u + 1) * 128], WD[i][:, fc, half * 512:(half + 1) * 512], fc == 0, fc == 3,
                                   [("HIDs", j2), ("WD", i, fc // 2)], [pk])
                            cp("act" if half == 0 else "dve", YS[yi][:, half * 512:(half + 1) * 512], bank(6 + half), [pk], [("YS", yi)])
                        ss = 2 * sl + u
                        dma("act", Yd[ss * 128:(ss + 1) * 128, :], YS[yi], [("YS", yi)], [("Yd", ss)])

                for k0 in range(min(3, n_slot_run)):
                    slot_load(k0)
                for sl in range(n_slot_run + 1):
                    if sl < n_slot_run:
                        tile_T(sl)
                    if sl >= 1:
                        tile_D(sl - 1)
                    if sl < n_slot_run:
                        tile_U(sl)
                    nxt = sl + 3 if sl % 2 == 1 else sl + 1
                    if sl >= 1 and 3 <= nxt < n_slot_run:
                        slot_load(nxt)

                YG = [[A.v(O_TA + 8192 * i + 4096 * k, [128, D], F32) for k in range(2)] for i in range(2)]
                YDK = [("Yd", ss) for ss in range(2 * n_slot_run)]
                for t in range(NT):
                    i = t % 2
                    for k in range(2):
                        P.dma("pool", lambda e, k=k, t=t, i=i: e.indirect_dma_start(
                            out=YG[i][k], out_offset=None, in_=Yd, in_offset=bass.IndirectOffsetOnAxis(ap=S12i[:, k, t:t + 1], axis=0)),
                            YDK, [("YG", i, k)])
                        stt(h[:, t, :], YG[i][k], W12[:, k, t:t + 1], h[:, t, :], ALU.mult, ALU.add, [("YG", i, k), ("h", t)], [("h", t)])
                if debug_stage in (5, 7):
                    dbg_out("h2", h, [128, NT, D], HK)
                    raise _Stop()

            if not SPARSE:
                P.barrier()
                WU = [A.v(O_MIX + 16384 * i, [128, 8, 1024]) for i in range(2)]
                WD = [A.v(O_W + 8192 * i, [128, 4, D]) for i in range(2)]
                HID = [A.v(O_TB + 4096 * i, [128, 4, 512]) for i in range(2)]
                SA = [A.v(O_TB + 8192 + 1024 * i, [128, 512]) for i in range(2)]
                n_exp_run = N_EXP if debug_stage != 5 else 2
                ub = 0
                db = 0
                sai = 0
                for ex in range(n_exp_run):
                    wi = ex % 2
                    dma("pool", WU[wi], w_up[ex].rearrange("(k p) n -> p k n", p=128), (), [("WU", wi)])
                    dma("pool", WD[wi], w_down[ex].rearrange("(k p) n -> p k n", p=128), (), [("WD", wi)])
                    for tg in range(4):
                        hi_ = (ex * 4 + tg) % 2
                        hk = ("HID", hi_)
                        for fc in range(4):
                            b0 = 2 * (ub % 2); ub += 1
                            for part, fcol in enumerate((fc, fc + 4)):
                                pk = ("ps", b0 + part)
                                for kc in range(8):
                                    mm(bank(b0 + part), WU[wi][:, kc, fcol * 128:(fcol + 1) * 128], XT[:, kc, tg * 512:(tg + 1) * 512],
                                       kc == 0, kc == 7, [("WU", wi)] + XTK[tg * 4:(tg + 1) * 4], [pk])
                            s_ = sai % 2; sai += 1
                            act(SA[s_], bank(b0), AF.Silu, [("ps", b0)], [("SA", s_)])
                            tt("dve", HID[hi_][:, fc, :], SA[s_], bank(b0 + 1), ALU.mult, [("SA", s_), ("ps", b0 + 1)], [hk])
                        for tt_ in range(4):
                            t = 4 * tg + tt_
                            for half in range(2):
                                bnk = 4 + (db % 4); db += 1
                                pk = ("ps", bnk)
                                for fc in range(4):
                                    mm(bank(bnk), HID[hi_][:, fc, tt_ * 128:(tt_ + 1) * 128], WD[wi][:, fc, half * 512:(half + 1) * 512],
                                       fc == 0, fc == 3, [hk, ("WD", wi)], [pk])
                                hv = h[:, t, half * 512:(half + 1) * 512]
                                stt(hv, bank(bnk), gate_t[:, t, ex:ex + 1], hv, ALU.mult, ALU.add, [pk, ("gate", t), ("h", t)], [("h", t)])

                if debug_stage == 5:
                    dbg_out("h2", h, [128, NT, D], HK)
                    raise _Stop()

            P.barrier()
            WG = A.v(O_W, [128, 8, D])
            WP = A.v(O_MIX, [128, 2, D])
            PTT = A.v(O_MIX + 4096, [128, 2, S])
            SG = [A.v(O_TB + 4096 * i, [128, D], F32) for i in range(2)]
            TM = [A.v(O_TB + 8192 + 4096 * i, [128, D], F32) for i in range(2)]
            ptile = [A.v(O_TB + 16384 + 1024 * i, [128, 256], F32) for i in range(2)]
            dma("pool", WG, w_pg.rearrange("(k p) n -> p k n", p=128), (), ["WG"])
            dma("pool", WP, w_pp.rearrange("(k p) n -> p k n", p=128), (), ["WP"])
            dma("sync", gbc, dram_pbcast(g_ple, 128, D), (), ["gbc"])
            def s5A(t):
                i = t % 2
                dma("sync", ptile[i], p_in[t * 128:(t + 1) * 128, :], (), [("ptile", i)])
                norm_stats(h[:, t, :], ("h", t), i)

            def s5B(t):
                i = t % 2
                norm_tr(t, i, 0)
                pk6 = ("ps", 6)
                for c in range(2):
                    tr32(bank(6)[:, c * 128:(c + 1) * 128], ptile[i][:, c * 128:(c + 1) * 128], [("ptile", i), "ident"], [pk6], inc=(c == 1))
                cp("act", PTT[:, :, t * 128:(t + 1) * 128], bank(6)[:, 0:256].rearrange("p (a b) -> p a b", a=2), [pk6], [("PTT", t)])

            def s5C(t):
                i = t % 2
                for half in range(2):
                    gk_ = ("ps", 2 + half)
                    for kc in range(8):
                        mm(bank(2 + half), XT[:, kc, t * 128:(t + 1) * 128], WG[:, kc, half * 512:(half + 1) * 512], kc == 0, kc == 7,
                           ["WG", ("XT", t)], [gk_])
                    ppk = ("ps", 4 + half)
                    for c in range(2):
                        mm(bank(4 + half), PTT[:, c, t * 128:(t + 1) * 128], WP[:, c, half * 512:(half + 1) * 512], c == 0, c == 1,
                           ["WP", ("PTT", t)], [ppk])
                    hs = slice(half * 512, (half + 1) * 512)
                    act(SG[i][:, hs], bank(2 + half), AF.Sigmoid, [gk_], [("SG", i, half)])
                    tt("dve", TM[i][:, hs], SG[i][:, hs], bank(4 + half), ALU.mult, [("SG", i, half), ppk], [("TM", i, half)])
                    tt("pool", TM[i][:, hs], TM[i][:, hs], h[:, t, hs], ALU.add, [("TM", i, half), ("h", t)], [("TM", i, half)])
                dma("sync", out[t * 128:(t + 1) * 128, :], TM[i], [("TM", i, 0), ("TM", i, 1)], (), is_output=True)

            skew([s5A, s5B, s5C], NT)


        try:
            body()
        except _Stop:
            pass
        P.finish()
        P.emit()
        n_ops = P.n_ops
    return nc, dbg, n_ops


def make_consts():
    cm = np.zeros((128, NCM), np.float32)
    cm[:, 0:128] = np.eye(128, dtype=np.float32)
    bd = np.zeros((128, 128), np.float32)
    bd[0:64, 0:64] = 1.0
    bd[64:128, 64:128] = 1.0
    cm[:, 128:256] = bd
    rot = np.zeros((128, 128), np.float32)
    for hb in (0, 64):
        for i in range(8):
            rot[hb + i + 8, hb + i] = -1.0
            rot[hb + i, hb + i + 8] = 1.0
    cm[:, 256:384] = rot
    b = np.arange(128)[:, None]
    a = np.arange(128)[None, :]
    cm[:, 384:512] = (b <= a).astype(np.float32)
    cm[:, 512:640] = (b >= a).astype(np.float32)
    for n in range(4):
        a3 = np.arange(32)[None, :]
        cm[:, 640 + 32 * n:672 + 32 * n] = (b <= 32 * n + a3).astype(np.float32)
    cm[:, 768:896] = (b < a).astype(np.float32)
    cvec = np.zeros((128, 96), np.float32)
    cvec[:, 0] = np.arange(128)
    cvec[:, 1:65] = np.arange(64)[None, :]
    cvec[:, 65:81] = 128.0 * np.arange(16)[None, :]
    ident = np.eye(128, dtype=np.float32)
    inv = (np.float32(500000.0) ** (-np.arange(0, 16, 2, dtype=np.float32) / np.float32(16))).astype(np.float32)
    invf = np.zeros((128, 1), np.float32)
    for p in range(128):
        d = p % 64
        if d < 16:
            invf[p, 0] = inv[d % 8]
    return cm, ident, invf, cvec


_CACHE = {}


def _get_program(debug_stage=None):
    if debug_stage not in _CACHE:
        _CACHE[debug_stage] = build_program(debug_stage)
    return _CACHE[debug_stage]


def make_in_maps(x, p, positions, g_mix, w_in, b_f, qn_a, kn_a, qn_b, kn_b, w_o, g_ffn, w_rg, b_rg, w_re, b_re,
                 w_up, w_down, g_ple, w_ple_gate, w_ple_proj, n_cores=8):
    f = lambda a: np.ascontiguousarray(np.asarray(a), dtype=np.float32)
    cm, ident, invf, cvec = make_consts()
    w_r = np.concatenate([f(w_rg)[0], f(w_re)[0].transpose(1, 0, 2).reshape(D, 32)], axis=1)
    b_r = np.concatenate([f(b_rg)[0].reshape(4), f(b_re)[0].reshape(32)])
    shared = {
        "g_mix": f(g_mix)[0], "w_in": f(w_in)[0], "b_f": f(b_f)[0],
        "qn_a": f(qn_a)[0], "kn_a": f(kn_a)[0], "qn_b": f(qn_b)[0], "kn_b": f(kn_b)[0],
        "w_o": f(w_o)[0], "g_ffn": f(g_ffn)[0], "w_r": np.ascontiguousarray(w_r), "b_r": np.ascontiguousarray(b_r),
        "w_up_r": np.ascontiguousarray(f(w_up)[0].reshape(N_EXP, 8, 128, 1024).transpose(0, 2, 1, 3)).reshape(N_EXP * 128 * 4, 2048),
        "w_down_r": np.ascontiguousarray(f(w_down)[0].reshape(N_EXP, 4, 128, D).transpose(0, 2, 1, 3)).reshape(N_EXP * 128 * 2, 2048),
        "g_ple": f(g_ple)[0],
        "w_pg": f(w_ple_gate)[0], "w_pp": f(w_ple_proj)[0],
        "cmat": cm, "ident": ident, "invf": invf, "cvec": cvec,
    }
    xs = f(x)
    ps = f(p)[0]
    pos = np.ascontiguousarray(np.asarray(positions), dtype=np.int32)
    in_maps = []
    for b in range(n_cores):
        m = dict(shared)
        m["x"] = xs[b]
        m["p"] = ps[b]
        m["pos"] = pos[b]
        in_maps.append(m)
    return in_maps


def kernel(**inputs):
    nc, _, _ = _get_program(None)
    in_maps = make_in_maps(**inputs)
    res = run_bass_kernel_spmd(nc, in_maps, core_ids=list(range(8)))
    return np.stack([np.asarray(r["out"]) for r in res.results], axis=0).astype(np.float32)
```

```python
import math
from contextlib import ExitStack

import numpy as np
import concourse.bass as bass
import concourse.mybir as mybir
from concourse.bass_utils import run_bass_kernel_spmd

F32 = mybir.dt.float32
BF16 = mybir.dt.bfloat16
I32 = mybir.dt.int32
AF = mybir.ActivationFunctionType
ALU = mybir.AluOpType
AX = mybir.AxisListType

ENGS = ("sync", "pe", "act", "dve", "pool")

S = 2048
D = 1024
NT = 16
EPS = 1e-6
N_EXP = 32
BIG = 1.0e30


class Prog:
    def __init__(self, nc, stack, n_dma_sems=8):
        self.nc = nc
        self.streams = {e: [] for e in ENGS}
        self.esem = {}
        for e in ("pe", "act", "dve", "pool"):
            self.esem[e] = stack.enter_context(nc.semaphore("s_" + e))
        self.ecount = {e: 0 for e in ("pe", "act", "dve", "pool")}
        self.dsems, self.dcount, self.drr = {}, {}, {}
        for q, nsem in (("sync", 12), ("act", 8), ("pool", 40)):
            self.dsems[q] = [stack.enter_context(nc.semaphore(f"d_{q}{i}")) for i in range(nsem)]
            self.dcount[q] = [0] * nsem
            self.drr[q] = 0
        self.known = {e: {} for e in ENGS}
        self.last_w = {}
        self.readers = {}
        self.out_tokens = []
        self.n_ops = 0

    def _deps(self, eng, reads, writes):
        deps = []
        for k in reads:
            t = self.last_w.get(k)
            if t is not None:
                deps.append(t)
        for k in writes:
            t = self.last_w.get(k)
            if t is not None:
                deps.append(t)
            deps.extend(self.readers.get(k, ()))
        kn = self.known[eng]
        best = {}
        for (sem, val, seng) in deps:
            if seng == "pe" and eng == "pe":
                continue
            if kn.get(id(sem), 0) >= val:
                continue
            if id(sem) not in best or best[id(sem)][1] < val:
                best[id(sem)] = (sem, val)
        for sem, val in best.values():
            kn[id(sem)] = val
        return list(best.values())

    def _commit(self, token, reads, writes):
        for k in writes:
            self.last_w[k] = token
            self.readers[k] = []
        for k in reads:
            if k in writes:
                continue
            self.readers.setdefault(k, []).append(token)

    def op(self, eng, fn, reads=(), writes=(), inc=True):
        reads, writes = tuple(reads), tuple(writes)
        waits = self._deps(eng, reads, writes)
        if inc:
            self.ecount[eng] += 1
            token = (self.esem[eng], self.ecount[eng], eng)
        else:
            token = (self.esem[eng], self.ecount[eng] + 1, eng)
        self.streams[eng].append((waits, fn, (self.esem[eng], 1) if inc else None))
        self._commit(token, reads, writes)
        self.n_ops += 1
        return token

    def dma(self, q, fn, reads=(), writes=(), is_output=False):
        reads, writes = tuple(reads), tuple(writes)
        waits = self._deps(q, reads, writes)
        j = self.drr[q]
        self.drr[q] = (j + 1) % len(self.dsems[q])
        sem = self.dsems[q][j]
        prev = self.dcount[q][j]
        if prev > 0 and self.known[q].get(id(sem), 0) < prev:
            self.known[q][id(sem)] = prev
            waits = [w for w in waits if w[0] is not sem] + [(sem, prev)]
        self.dcount[q][j] += 16
        token = (sem, self.dcount[q][j], "dma_" + q)
        self.streams[q].append((waits, fn, (sem, 16)))
        self._commit(token, reads, writes)
        if is_output:
            self.out_tokens.append(token)
        self.n_ops += 1
        return token

    def barrier(self):
        toks = []
        for e in ("pe", "act", "dve", "pool"):
            if self.ecount[e] > 0:
                toks.append((self.esem[e], self.ecount[e]))
        for q in ("sync", "act", "pool"):
            for j, s in enumerate(self.dsems[q]):
                if self.dcount[q][j] > 0:
                    toks.append((s, self.dcount[q][j]))
        for e in ENGS:
            waits = []
            for sem, val in toks:
                if self.known[e].get(id(sem), 0) >= val:
                    continue
                self.known[e][id(sem)] = val
                waits.append((sem, val))
            if waits:
                self.streams[e].append((waits, None, None))
        self.last_w = {}
        self.readers = {}

    def finish(self):
        best = {}
        for sem, val, _ in self.out_tokens:
            if id(sem) not in best or best[id(sem)][1] < val:
                best[id(sem)] = (sem, val)
        self.streams["sync"].append((list(best.values()), None, None))

    def emit(self):
        streams = self.streams

        def run(name, e):
            for waits, fn, inc in streams[name]:
                for sem, val in waits:
                    e.wait_ge(sem, val)
                if fn is not None:
                    ins = fn(e)
                    if inc is not None:
                        ins.then_inc(inc[0], inc[1])

        with self.nc.Block() as block:
            @block.sync
            def _(e):
                run("sync", e)

            @block.tensor
            def _(e):
                run("pe", e)

            @block.scalar
            def _(e):
                run("act", e)

            @block.vector
            def _(e):
                run("dve", e)

            @block.gpsimd
            def _(e):
                run("pool", e)


def strided(ap2d, start, step, count):
    return bass.AP(ap2d.tensor, ap2d.offset + start, [list(ap2d.ap[0]), [step, count]])


def bcast_mid(ap2d, reps):
    return bass.AP(ap2d.tensor, ap2d.offset, [list(ap2d.ap[0]), [0, reps], list(ap2d.ap[1])])


def bcast_last(ap2d, reps):
    return bass.AP(ap2d.tensor, ap2d.offset, [list(ap2d.ap[0]), list(ap2d.ap[1]), [0, reps]])


def dram_pbcast(ap1d, parts, n):
    return bass.AP(ap1d.tensor, ap1d.offset, [[0, parts], [1, n]])


class _Stop(Exception):
    pass


class Arena:
    def __init__(self, t, nbytes):
        self.t = t
        self.nbytes = nbytes

    def v(self, off, shape, dtype=BF16, parts=None):
        es = 2 if dtype == BF16 else 4
        n = 1
        for s in shape[1:]:
            n *= s
        nb = n * es
        assert off % 4 == 0 and off + nb <= self.nbytes, (off, nb, self.nbytes)
        p0, p1 = (0, shape[0]) if parts is None else parts
        a = self.t[p0:p1, off // 2:(off + nb) // 2]
        if dtype != BF16:
            a = a.bitcast(dtype)
        if len(shape) == 3:
            a = a.rearrange("p (a b) -> p a b", a=shape[1])
        elif len(shape) == 4:
            a = a.rearrange("p (a b c) -> p a b c", a=shape[1], b=shape[2])
        return a


ARENA_BYTES = 200704
O_XT = 0
O_MIX = 32768
O_R1 = 65536
O_W = 131072
O_TA = 147456
O_TB = 176128

NCM = 128 * 6 + 4 * 32
SPARSE = True
NSLOT = 48
NTILE_RUN = 47
CAP = 256
NSUB = 96


def build_program(debug_stage=None):
    nc = bass.Bass("TRN2", target_bir_lowering=False)
    dr = {}

    def din(name, shape, dt=F32):
        dr[name] = nc.dram_tensor(name, list(shape), dt, kind="ExternalInput").ap()
        return dr[name]

    x = din("x", [S, D])
    p_in = din("p", [S, 256])
    pos = din("pos", [S], I32)
    g_mix = din("g_mix", [D])
    w_in = din("w_in", [D, 3080])
    b_f = din("b_f", [8])
    qn_a = din("qn_a", [64]); kn_a = din("kn_a", [64]); qn_b = din("qn_b", [64]); kn_b = din("kn_b", [64])
    w_o = din("w_o", [D, D])
    g_ffn = din("g_ffn", [D])
    w_r = din("w_r", [D, 36])
    b_r = din("b_r", [36])
    g_ple = din("g_ple", [D])
    w_pg = din("w_pg", [D, D])
    w_pp = din("w_pp", [256, D])
    cmat_d = din("cmat", [128, NCM])
    ident_d = din("ident", [128, 128])
    invf_d = din("invf", [128, 1])
    cvec_d = din("cvec", [128, 96])
    w_up_r = din("w_up_r", [N_EXP * 128 * 4, 2048])
    w_down_r = din("w_down_r", [N_EXP * 128 * 2, 2048])
    out = nc.dram_tensor("out", [S, D], F32, kind="ExternalOutput").ap()
    cps = nc.dram_tensor("cps", [2, 3, 8, S], BF16).ap()
    mrows = nc.dram_tensor("mrows", [S, D], BF16).ap()
    tos = nc.dram_tensor("tos", [NSUB * 128, 16], I32).ap()
    Yd = nc.dram_tensor("Yd", [NSUB * 128, D], F32).ap()

    dbg = {}

    with ExitStack() as st:
        P = Prog(nc, st)
        arena_t = st.enter_context(nc.sbuf_tensor("arena", [128, ARENA_BYTES // 2], BF16))
        A = Arena(arena_t, ARENA_BYTES)
        cmat = st.enter_context(nc.sbuf_tensor("cmat_sb", [128, NCM], BF16))
        ident = st.enter_context(nc.sbuf_tensor("ident_sb", [128, 128], F32))
        smallf = st.enter_context(nc.sbuf_tensor("smallf", [128, 256], F32))
        gate_t = st.enter_context(nc.sbuf_tensor("gate_sb", [128, NT, 32], F32))
        psum = st.enter_context(nc.psum_tensor("psum", [128, 4096], F32))

        def bank(b, n=1):
            return psum[:, b * 512:(b + n) * 512]

        identb = cmat[:, 0:128]
        BD = cmat[:, 128:256]
        ROT = cmat[:, 256:384]
        maskC = cmat[:, 384:512]
        maskP = cmat[:, 512:640]
        mask3 = [cmat[:, 640 + 32 * n: 672 + 32 * n] for n in range(4)]
        LST = cmat[:, 768:896]
        cvec = st.enter_context(nc.sbuf_tensor("cvec_sb", [128, 96], F32))
        M2all = st.enter_context(nc.sbuf_tensor("m2all_sb", [128, NT, 32], F32))
        W12 = st.enter_context(nc.sbuf_tensor("w12_sb", [128, 2, NT], F32))
        S12i = st.enter_context(nc.sbuf_tensor("s12i_sb", [128, 2, NT], I32))
        idxU = st.enter_context(nc.sbuf_tensor("idxu_sb", [128, NSLOT, 4], I32))
        idxD = st.enter_context(nc.sbuf_tensor("idxd_sb", [128, NSLOT, 2], I32))
        TOK = st.enter_context(nc.sbuf_tensor("tok_sb", [128, NSUB], I32))
        tokrep = st.enter_context(nc.sbuf_tensor("tokrep_sb", [128, NT, 16], I32))
        onesm = st.enter_context(nc.sbuf_tensor("onesm_sb", [128, 128], BF16))
        bnd_reg = st.enter_context(nc.gpsimd.register("bnd"))
        P.streams["pool"].append(([], lambda e: e.reg_mov(bnd_reg, N_EXP * 128 * 4 - 1), None))

        invf = smallf[:, 0:1]
        gq_a = smallf[:, 1:2]; gk_a = smallf[:, 2:3]; gq_b = smallf[:, 3:4]; gk_b = smallf[:, 4:5]
        negbf = smallf[0:8, 5:6]
        ss_col = smallf[:, 8:9]
        rs_col = smallf[:, 9:10]
        rt = smallf[:, 16:80]
        bias_r = smallf[:, 96:132]
        L_r = smallf[:, 136:172]
        lem2 = smallf[:, 176:208]
        msk1 = smallf[:, 208:240]

        XT = A.v(O_XT, [128, 8, S])
        MIXT = A.v(O_MIX, [128, 8, S])
        h = A.v(O_R1, [128, NT, D], F32)

        xt = [A.v(O_TA + 4096 * i, [128, D], F32) for i in range(2)]
        sqf = A.v(O_TA + 8192, [128, D], F32)
        hn = [A.v(O_TA + 12288 + 4096 * i, [128, D], F32) for i in range(2)]
        gbc = A.v(O_TA + 20480, [128, D], F32)
        m32 = A.v(O_TA + 24576, [128, 8, 128], F32)

        def mm(o, lhsT, rhs, start, stop, r, w, inc=None):
            P.op("pe", lambda e: e.matmul(o, lhsT, rhs, start=start, stop=stop, skip_group_check=True),
                 r, w, inc=(stop if inc is None else inc))

        def tr32(o, in_, r, w, inc=True):
            P.op("pe", lambda e: e.transpose(o, in_, ident[:, :]), r, w, inc=inc)

        def act(o, in_, func, r, w, bias=None, scale=None):
            kw = {}
            if bias is not None:
                kw["bias"] = bias
            if scale is not None:
                kw["scale"] = scale
            P.op("act", lambda e: e.activation(out=o, in_=in_, func=func, **kw), r, w)

        def tt(eng, o, in0, in1, op, r, w):
            P.op(eng, lambda e: e.tensor_tensor(out=o, in0=in0, in1=in1, op=op), r, w)

        def ts(eng, o, in0, s1, s2, op0, op1, r, w):
            if op1 is None:
                P.op(eng, lambda e: e.tensor_scalar(out=o, in0=in0, scalar1=s1, scalar2=None, op0=op0), r, w)
            else:
                P.op(eng, lambda e: e.tensor_scalar(out=o, in0=in0, scalar1=s1, scalar2=s2, op0=op0, op1=op1), r, w)

        def stt(o, in0, scalar, in1, op0, op1, r, w):
            P.op("dve", lambda e: e.scalar_tensor_tensor(out=o, in0=in0, scalar=scalar, in1=in1, op0=op0, op1=op1), r, w)

        def cp(eng, o, in_, r, w):
            if eng == "act":
                P.op("act", lambda e: e.activation(out=o, in_=in_, func=AF.Copy), r, w)
            else:
                P.op(eng, lambda e: e.tensor_copy(out=o, in_=in_), r, w)

        def red(o, in_, op, r, w):
            P.op("dve", lambda e: e.tensor_reduce(out=o, in_=in_, axis=AX.X, op=op), r, w)

        def recip(o, in_, r, w):
            P.op("dve", lambda e: e.reciprocal(out=o, in_=in_), r, w)

        def memset(eng, ap, val, w):
            P.op(eng, lambda e: e.memset(ap, val), (), w)

        def dma(q, o, in_, r, w, is_output=False):
            P.dma(q, lambda e: e.dma_start(out=o, in_=in_), r, w, is_output=is_output)

        def dbg_out(name, ap_sb, shape, keys, dt=F32):
            d = nc.dram_tensor("dbg_" + name, list(shape), dt, kind="ExternalOutput").ap()
            dbg[name] = d
            dma("sync", d, ap_sb, keys, (), is_output=True)

        def body():
            dma("pool", cmat[:, :], cmat_d, (), ["cmat"])
            dma("sync", ident[:, :], ident_d, (), ["ident"])
            dma("sync", invf, invf_d, (), ["sm_invf"])
            dma("sync", cvec[:, :], cvec_d, (), ["cvec"])
            memset("pool", onesm[:, :], 1.0, ["onesm"])
            for ci, (col, src) in enumerate(((gq_a, qn_a), (gk_a, kn_a), (gq_b, qn_b), (gk_b, kn_b))):
                s2 = src.rearrange("(p o) -> p o", o=1)
                dma("sync", col[0:64, :], s2, (), [("sm_g", ci, 0)])
                dma("sync", col[64:128, :], s2, (), [("sm_g", ci, 1)])
            dma("sync", negbf, b_f.rearrange("(p o) -> p o", o=1), (), ["sm_bf"])
            dma("sync", bias_r, dram_pbcast(b_r, 128, 36), (), ["sm_br"])
            ts("dve", smallf[:, 2:3], smallf[:, 2:3], 0.125, None, ALU.mult, None, [("sm_g", 1, 0), ("sm_g", 1, 1)], ["small"])
            ts("dve", smallf[:, 4:5], smallf[:, 4:5], 0.125, None, ALU.mult, None, [("sm_g", 3, 0), ("sm_g", 3, 1)], ["small"])
            ts("dve", negbf, negbf, -1.0, None, ALU.mult, None, ["sm_bf"], ["small"])
            SMK = ["small", "sm_invf", "sm_br"] + [("sm_g", ci, hf) for ci in range(4) for hf in range(2)]

            def norm_stats(src, src_key, i):
                hb = hn[i]
                hk = ("hn", i)
                act(sqf, src, AF.Square, [src_key], ["sqf"])
                red(ss_col, sqf, ALU.add, ["sqf"], ["ss"])
                act(rs_col, ss_col, AF.Ln, ["ss"], ["rs"], bias=EPS, scale=1.0 / D)
                act(rs_col, rs_col, AF.Exp, ["rs"], ["rs"], scale=-0.5)
                stt(hb, src, rs_col, gbc, ALU.mult, ALU.mult, [src_key, "rs", "gbc"], [hk])

            def norm_tr(t, i, pb0, m32buf=None, m32key=None):
                hb = hn[i]
                hk = ("hn", i)
                pk = [("ps", pb0), ("ps", pb0 + 1)]
                for kc in range(8):
                    tr32(psum[:, pb0 * 512 + kc * 128: pb0 * 512 + (kc + 1) * 128], hb[:, kc * 128:(kc + 1) * 128],
                         [hk, "ident"], [pk[kc // 4]], inc=(kc % 4 == 3))
                pv = bank(pb0, 2).rearrange("p (a b) -> p a b", a=8)
                xk = ("XT", t)
                cp("act", XT[:, 0:4, t * 128:(t + 1) * 128], pv[:, 0:4, :], [pk[0]], [xk])
                cp("dve", XT[:, 4:8, t * 128:(t + 1) * 128], pv[:, 4:8, :], [pk[1]], [xk])
                if m32buf is not None:
                    cp("dve", m32buf[:, 0:4, :], pv[:, 0:4, :], [pk[0]], [m32key])
                    cp("act", m32buf[:, 4:8, :], pv[:, 4:8, :], [pk[1]], [m32key])

            def skew(phases, n):
                for step in range(n + len(phases) - 1):
                    for k_, f_ in enumerate(phases):
                        t_ = step - k_
                        if 0 <= t_ < n:
                            f_(t_)

            dma("sync", gbc, dram_pbcast(g_mix, 128, D), (), ["gbc"])
            def s1A(t):
                i = t % 2
                dma("sync", xt[i], x[t * 128:(t + 1) * 128, :], (), [("xt", i)])
                norm_stats(xt[i], ("xt", i), i)

            def s1B(t):
                norm_tr(t, t % 2, 2 * (t % 2))

            skew([s1A, s1B], NT)
            XTK = [("XT", t) for t in range(NT)]

            if debug_stage == 1:
                dbg_out("XT", XT, [128, 8, S], XTK, BF16)
                raise _Stop()

            COS = A.v(O_R1 + 49152, [128, S])
            SIN = A.v(O_R1 + 53248, [128, S])
            posi = A.v(O_MIX, [128, S], I32)
            ang = A.v(O_MIX + 8192, [128, S], F32)
            kf = A.v(O_MIX + 16384, [128, S], F32)
            ki = A.v(O_MIX + 24576, [128, S], I32)
            dma("sync", posi, dram_pbcast(pos, 128, S), (), ["posi"])
            C1 = 6.28125
            C2 = float(2 * math.pi - 6.28125)

            def sin_table(dst, shift):
                cp("dve", ang, posi, ["posi"], ["ang"])
                if shift == 0.0:
                    ts("dve", ang, ang, invf, None, ALU.mult, None, ["ang", "sm_invf"], ["ang"])
                else:
                    ts("dve", ang, ang, invf, shift, ALU.mult, ALU.add, ["ang", "sm_invf"], ["ang"])
                ts("dve", ki, ang, float(1.0 / (2 * math.pi)), None, ALU.mult, None, ["ang"], ["ki"])
                cp("dve", kf, ki, ["ki"], ["kf"])
                stt(ang, kf, -C1, ang, ALU.mult, ALU.add, ["kf", "ang"], ["ang"])
                stt(ang, kf, -C2, ang, ALU.mult, ALU.add, ["kf", "ang"], ["ang"])
                ts("dve", kf, ang, float(math.pi), float(-2 * math.pi), ALU.is_gt, ALU.mult, ["ang"], ["kf"])
                tt("dve", ang, ang, kf, ALU.add, ["ang", "kf"], ["ang"])
                ts("dve", kf, ang, float(-math.pi), float(2 * math.pi), ALU.is_lt, ALU.mult, ["ang"], ["kf"])
                tt("dve", ang, ang, kf, ALU.add, ["ang", "kf"], ["ang"])
                ts("dve", ang, ang, 3.14159, -3.14159, ALU.min, ALU.max, ["ang"], ["ang"])
                act(dst, ang, AF.Sin, ["ang"], [("tab", shift)])

            sin_table(SIN, 0.0)
            sin_table(COS, float(math.pi / 2))
            TABK = [("tab", 0.0), ("tab", float(math.pi / 2))]

            wf = A.v(O_W + 12288, [128, 8, 8])
            dma("pool", wf, w_in[:, 3072:3080].rearrange("(k p) n -> p k n", p=128), (), ["wf"])
            SP = A.v(O_MIX + 16384, [8, S], F32)
            CS = A.v(O_MIX + 24576, [8, S], F32)
            oc_ = smallf[0:8, 10:11]
            ONES8 = bass.AP(oc_.tensor, oc_.offset, [list(oc_.ap[0]), [0, S]])
            PRT = A.v(O_R1 + 59392, [8, S])
            E1 = A.v(O_R1 + 57344, [8, 512], F32)
            memset("pool", smallf[0:8, 10:11], 1.0, ["ones8"])
            for tg in range(4):
                pk = ("ps", tg % 2)
                for kc in range(8):
                    mm(bank(tg % 2)[0:8, :], wf[:, kc, :], XT[:, kc, tg * 512:(tg + 1) * 512], kc == 0, kc == 7,
                       ["wf"] + XTK[tg * 4:(tg + 1) * 4], [pk])
                act(E1, bank(tg % 2)[0:8, :], AF.Exp, [pk, "small"], ["E1"], bias=negbf, scale=-1.0)
                act(SP[:, tg * 512:(tg + 1) * 512], E1, AF.Ln, ["E1"], ["SP"], bias=1.0, scale=1.0)
            P.op("dve", lambda e: e.tensor_tensor_scan(out=CS, data0=ONES8, data1=SP, initial=0.0, op0=ALU.mult, op1=ALU.add),
                 ["ones8", "SP"], ["CS"])
            R1 = SP
            for j in range(3):
                src = CS if j == 0 else R1
                cp("dve", PRT, src, ["CS", "SP"], ["PRT"])
                dma("sync", cps[0, j, :, :], PRT, ["PRT"], [("cps", 0, j)])
                if j < 2:
                    tt("dve", R1, src, PRT, ALU.subtract, ["CS", "SP", "PRT"], ["SP"])
                ts("dve", PRT, PRT, -1.0, None, ALU.mult, None, ["PRT"], ["PRT"])
                dma("sync", cps[1, j, :, :], PRT, ["PRT"], [("cps", 1, j)])
            if debug_stage == 3:
                dbg_out("CS", CS, [8, S], ["CS"])

            PT = [A.v(O_TB + 1024 * i, [128, 512]) for i in range(4)]
            sqb = [A.v(O_TB + 4096 + 1024 * i, [128, 512]) for i in range(2)]
            lnv = [A.v(O_TB + 6144 + 2048 * i, [128, 512], F32) for i in range(2)]
            rstd = [A.v(O_TB + 10240 + 2048 * i, [128, 512], F32) for i in range(2)]
            qnb = [A.v(O_TB + 14336 + 1024 * i, [128, 512]) for i in range(2)]
            t1b = [A.v(O_TB + 16384 + 2048 * i, [128, 512], F32) for i in range(2)]
            t2b = [A.v(O_TB + 20480 + 2048 * i, [128, 512], F32) for i in range(2)]
            Rb = [A.v(O_TA + 8192 + 2048 * i, [128, 512], F32) for i in range(2)]
            wsl = [A.v(O_W + 6144 * i, [128, 8, 384]) for i in range(2)]

            cnt = {"pt": 0, "set": 0, "sc": 0, "rb": 0}

            def load_wsl(i, cq, ck, cv):
                for j, c0 in enumerate((cq, ck, cv)):
                    src = w_in[:, c0:c0 + 128].rearrange("(k p) n -> p k n", p=128)
                    dma("pool", wsl[i][:, :, j * 128:(j + 1) * 128], src, (), [("wsl", i)])

            def project_T(i, j, tg, pbank):
                pk = ("ps", pbank)
                for kc in range(8):
                    mm(bank(pbank), wsl[i][:, kc, j * 128:(j + 1) * 128], XT[:, kc, tg * 512:(tg + 1) * 512],
                       kc == 0, kc == 7, [("wsl", i)] + XTK[tg * 4:(tg + 1) * 4], [pk])
                return pk

            def qk_norm_rstd(pbank, pk, sbank):
                s = cnt["set"] % 2
                cnt["set"] += 1
                act(sqb[s], bank(pbank), AF.Square, [pk], [("sqb", s)])
                sk = ("ps", sbank)
                mm(bank(sbank), BD, sqb[s], True, True, [("sqb", s), "cmat"], [sk])
                act(lnv[s], bank(sbank), AF.Ln, [sk], [("lnv", s)], bias=EPS, scale=1.0 / 64)
                act(rstd[s], lnv[s], AF.Exp, [("lnv", s)], [("rstd", s)], scale=-0.5)
                return s

            def vaug_build(VTb, vtk, VA, vak, tiles, pb_list):
                nb = len(tiles) // 4
                for b4 in range(nb):
                    pb = pb_list[b4 % len(pb_list)]
                    pk = ("ps", pb)
                    for tt_ in range(4):
                        mm(psum[:, pb * 512 + tt_ * 128: pb * 512 + (tt_ + 1) * 128], tiles[b4 * 4 + tt_], identb,
                           True, True, [vtk, "cmat"], [pk], inc=(tt_ == 3))
                    src = bank(pb).rearrange("p (t h d) -> p t h d", t=4, h=2)
                    eng = "dve" if b4 % 2 == 0 else "act"
                    cp(eng, VA[:, b4 * 4:(b4 + 1) * 4, :, 0:64], src, [pk], [vak])

            VA1 = A.v(O_R1, [128, NT, 2, 128])
            VA4 = A.v(O_R1 + 8192, [128, NT, 2, 128])
            VA16 = A.v(O_R1 + 16384, [128, NT, 2, 128])
            QTb = [A.v(O_R1 + 24576 + 4096 * i, [128, S]) for i in range(2)]
            KTb = [A.v(O_R1 + 32768 + 4096 * i, [128, S]) for i in range(2)]
            VTb = [A.v(O_R1 + 40960 + 4096 * i, [128, S]) for i in range(2)]
            for VA in (VA1, VA4, VA16):
                memset("pool", VA, 1.0, ["VA1", "VA4", "VA16"])

            def attn_norm_out(O_bank, ok, hp_chunk, hh, n):
                r = cnt["rb"] % 2
                cnt["rb"] += 1
                act(Rb[r][0:64, :], bank(O_bank)[64:128, :], AF.Ln, [ok], [("Rb", r)])
                act(Rb[r][0:64, :], Rb[r][0:64, :], AF.Exp, [("Rb", r)], [("Rb", r)], scale=-1.0)
                tt("dve", MIXT[64 * hh:64 * hh + 64, hp_chunk, n * 512:(n + 1) * 512], bank(O_bank)[0:64, :], Rb[r][0:64, :],
                   ALU.mult, [ok, ("Rb", r)], [("MIXT", hp_chunk, n)])

            def exp_mask(sbank, c0, c1, mask_ap3, mcols, eng):
                pi = cnt["pt"] % 4
                cnt["pt"] += 1
                pk = ("PT", pi)
                act(PT[pi][:, c0:c1], bank(sbank)[:, c0:c1], AF.Exp, [("ps", sbank)], [pk])
                if mask_ap3 is not None:
                    m0, m1 = mcols
                    view = PT[pi][:, m0:m1]
                    if len(mask_ap3.shape) == 3:
                        view = view.rearrange("p (a b) -> p a b", a=mask_ap3.shape[1])
                    tt(eng, view, view, mask_ap3, ALU.mult, [pk, "cmat"], [pk])
                return PT[pi], pk

            NSC = 4
            pending_norm = []
            NORM_DEFER = 3

            def run_pipeline(steps, LA=3):
                nst = len(steps)
                for s_ in range(nst + LA):
                    if s_ < nst:
                        qk_fn(steps[s_])
                    k_ = s_ - LA
                    if k_ >= 0:
                        ex_fn(steps[k_])
                        pv_fn(steps[k_])
                    for pn in list(pending_norm):
                        pn[0] -= 1
                        if pn[0] <= 0:
                            u_ = pn[1]
                            attn_norm_out(u_["Ob"], u_["ok"], u_["chunk"], u_["hh"], u_["n"])
                            pending_norm.remove(pn)
                for pn in list(pending_norm):
                    u_ = pn[1]
                    attn_norm_out(u_["Ob"], u_["ok"], u_["chunk"], u_["hh"], u_["n"])
                    pending_norm.remove(pn)

            def qk_fn(stp):
                sb_ = cnt["sc"] % NSC
                cnt["sc"] += 1
                stp["sb"] = sb_
                sk = ("ps", sb_)
                L = stp["qk_list"]
                for idx, (c0, c1, lhsT, rhs) in enumerate(L):
                    mm(bank(sb_)[:, c0:c1], lhsT, rhs, True, True, stp["qk_keys"], [sk], inc=(idx == len(L) - 1))

            def ex_fn(stp):
                stp["pt"], stp["ptk"] = exp_mask(stp["sb"], stp["c0"], stp["c1"], stp["mask"], stp["mcols"], stp["meng"])

            def pv_fn(stp):
                u = stp["u"]
                L = stp["pv_list"]
                for idx, (o_ap, lhsT, vak, p0, p1) in enumerate(L):
                    last = stp["last"] and idx == len(L) - 1
                    mm(o_ap, lhsT, stp["pt"][:, p0:p1], u["first"], last, [stp["ptk"], vak], [u["ok"]], inc=True)
                    u["first"] = False
                if stp["last"]:
                    pending_norm.append([NORM_DEFER, u])

            def new_unit(chunk, hh, n):
                uidx = cnt["unit"]
                cnt["unit"] += 1
                Ob = 4 + uidx % 4
                return dict(Ob=Ob, ok=("ps", Ob), first=True, hh=hh, n=n, chunk=chunk)

            def qk_chains(i, chains, rope):
                nch = len(chains)
                st_ = [dict() for _ in range(nch)]

                def phA(c):
                    j, tg, dst, dk, gain = chains[c]
                    pb = c % 3
                    pk = project_T(i, j, tg, pb)
                    s = c % 2
                    act(sqb[s], bank(pb), AF.Square, [pk], [("sqb", s)])
                    st_[c].update(pb=pb, pk=pk, s=s)

                def phB(c):
                    j, tg, dst, dk, gain = chains[c]
                    pb, pk, s = st_[c]["pb"], st_[c]["pk"], st_[c]["s"]
                    sbank = 3 + c % 2
                    sk = ("ps", sbank)
                    mm(bank(sbank), BD, sqb[s], True, True, [("sqb", s), "cmat"], [sk])
                    act(lnv[s], bank(sbank), AF.Ln, [sk], [("lnv", s)], bias=EPS, scale=1.0 / 64)
                    act(rstd[s], lnv[s], AF.Exp, [("lnv", s)], [("rstd", s)], scale=-0.5)
                    if rope:
                        stt(qnb[s], bank(pb), gain, rstd[s], ALU.mult, ALU.mult, [pk, ("rstd", s)] + SMK, [("qnb", s)])
                    else:
                        cs_ = slice(tg * 512, (tg + 1) * 512)
                        stt(dst[0:64, 0, cs_], bank(pb)[0:64, :], gain[0:64, :], rstd[s][0:64, :],
                            ALU.mult, ALU.mult, [pk, ("rstd", s)] + SMK, [dk])
                        stt(dst[0:64, 1, cs_], bank(pb)[64:128, :], gain[64:128, :], rstd[s][64:128, :],
                            ALU.mult, ALU.mult, [pk, ("rstd", s)] + SMK, [dk])

                def phC(c):
                    j, tg, dst, dk, gain = chains[c]
                    s = st_[c]["s"]
                    rb = 5 + c % 2
                    rk = ("ps", rb)
                    cs_ = slice(tg * 512, (tg + 1) * 512)
                    mm(bank(rb), ROT, qnb[s], True, True, [("qnb", s), "cmat"], [rk])
                    tt("dve", t1b[s], qnb[s], COS[:, cs_], ALU.mult, [("qnb", s), TABK[1]], [("t1", s)])
                    tt("dve", t2b[s], bank(rb), SIN[:, cs_], ALU.mult, [rk, TABK[0]], [("t2", s)])
                    tt("pool", dst[:, cs_], t1b[s], t2b[s], ALU.add, [("t1", s), ("t2", s)], [dk])

                for step in range(nch + 2):
                    if step < nch:
                        phA(step)
                    if 0 <= step - 1 < nch:
                        phB(step - 1)
                    if rope and 0 <= step - 2 < nch:
                        phC(step - 2)

            def v_proj(i, VT, vk_):
                for tg in range(4):
                    pb = tg % 3
                    pk = project_T(i, 2, tg, pb)
                    cp("act" if tg % 2 == 0 else "dve", VT[:, tg * 512:(tg + 1) * 512], bank(pb), [pk], [vk_])

            cnt["unit"] = 0
            load_wsl(0, 0, 512, 1024)
            for hp in range(4):
                i = hp % 2
                QT, KT, VT = QTb[i], KTb[i], VTb[i]
                qk_, kk_, vk_ = ("QT", i), ("KT", i), ("VT", i)
                chains = [(j, tg, dst, dk, gain) for j, (dst, dk, gain) in enumerate(((QT, qk_, gq_a), (KT, kk_, gk_a)))
                          for tg in range(4)]
                qk_chains(i, chains, True)
                v_proj(i, VT, vk_)
                vaug_build(VT, vk_, VA1, "VA1", [VT[:, 128 * t:128 * (t + 1)] for t in range(NT)], [3, 4, 6, 7])
                vaug_build(VT, vk_, VA4, "VA4", [strided(VT, 512 * (t // 4) + (t % 4), 4, 128) for t in range(NT)], [3, 4, 6, 7])
                vaug_build(VT, vk_, VA16, "VA16", [strided(VT, t, 16, 128) for t in range(NT)], [3, 4, 6, 7])
                if hp < 3:
                    load_wsl((hp + 1) % 2, 128 * (hp + 1), 512 + 128 * (hp + 1), 1024 + 128 * (hp + 1))
                else:
                    load_wsl(0, 1536, 2048, 2560)

                if debug_stage == 2 and hp == 0:
                    dbg_out("QT", QT, [128, S], [qk_], BF16)
                    dbg_out("KT", KT, [128, S], [kk_], BF16)
                    dbg_out("VT", VT, [128, S], [vk_], BF16)
                    dbg_out("VA4", VA4, [128, NT, 2, 128], ["VA4"], BF16)
                    dbg_out("COS", COS, [128, S], TABK, BF16)
                    dbg_out("SIN", SIN, [128, S], TABK, BF16)

                steps = []
                for n in range(4):
                    for hh in range(2):
                        u = new_unit(hp, hh, n)
                        Ob = u["Ob"]
                        Kh = KT[64 * hh:64 * hh + 64, :]
                        Qh = QT[64 * hh:64 * hh + 64, :]
                        qkk = [kk_, qk_]
                        steps.append(dict(u=u, qk_keys=qkk, c0=0, c1=512, mask=bcast_mid(maskC, 4), mcols=(0, 512), meng="dve", last=False,
                                          qk_list=[(128 * j, 128 * (j + 1), Kh[:, 128 * (4 * n + j):128 * (4 * n + j + 1)],
                                                    Qh[:, 128 * (4 * n + j):128 * (4 * n + j + 1)]) for j in range(4)],
                                          pv_list=[(bank(Ob)[:, 128 * j:128 * (j + 1)], VA1[:, 4 * n + j, hh, :], "VA1", 128 * j, 128 * (j + 1))
                                                   for j in range(4)]))
                        j0 = 1 if n == 0 else 0
                        steps.append(dict(u=u, qk_keys=qkk, c0=128 * j0, c1=512, mask=bcast_mid(maskP, 4 - j0), mcols=(128 * j0, 512), meng="dve", last=False,
                                          qk_list=[(128 * j, 128 * (j + 1), Kh[:, 128 * (4 * n + j - 1):128 * (4 * n + j)],
                                                    Qh[:, 128 * (4 * n + j):128 * (4 * n + j + 1)]) for j in range(j0, 4)],
                                          pv_list=[(bank(Ob)[:, 128 * j:128 * (j + 1)], VA1[:, 4 * n + j - 1, hh, :], "VA1", 128 * j, 128 * (j + 1))
                                                   for j in range(j0, 4)]))
                        for prev in (0, 1):
                            if prev and n == 0:
                                continue
                            steps.append(dict(u=u, qk_keys=qkk, c0=0, c1=512, mask=bcast_mid(maskP if prev else maskC, 4), mcols=(0, 512),
                                              meng="dve", last=False,
                                              qk_list=[(128 * r4, 128 * (r4 + 1), strided(Kh, 512 * (n - prev) + r4, 4, 128),
                                                        strided(Qh, 512 * n + r4, 4, 128)) for r4 in range(4)],
                                              pv_list=[(strided(bank(Ob), r4, 4, 128), VA4[:, 4 * (n - prev) + r4, hh, :], "VA4", 128 * r4, 128 * (r4 + 1))
                                                       for r4 in range(4)]))
                        steps.append(dict(u=u, qk_keys=qkk, c0=0, c1=512, mask=bcast_mid(mask3[n], 16), mcols=(0, 512), meng="dve", last=True,
                                          qk_list=[(32 * r, 32 * (r + 1), strided(Kh, r, 16, 128), strided(Qh, 512 * n + r, 16, 32)) for r in range(16)],
                                          pv_list=[(strided(bank(Ob), r, 16, 32), VA16[:, r, hh, :], "VA16", 32 * r, 32 * (r + 1)) for r in range(16)]))
                run_pipeline(steps)

            if debug_stage == 2:
                dbg_out("MIXA", MIXT[:, 0:4, :], [128, 4, S], [("MIXT", c, n) for c in range(4) for n in range(4)], BF16)
                raise _Stop()

            P.barrier()
            QP = [A.v(O_R1 + 8192 * i, [128, 2, S]) for i in range(2)]
            KP = [A.v(O_R1 + 16384 + 8192 * i, [128, 2, S]) for i in range(2)]
            VTB = [A.v(O_R1 + 32768 + 4096 * i, [128, S]) for i in range(2)]
            VAB = [A.v(O_R1 + 40960 + 8192 * i, [128, NT, 2, 128]) for i in range(2)]
            for i in range(2):
                memset("pool", VAB[i], 1.0, [("VAB", i)])
                memset("pool", QP[i][64:70, :, :], 1.0, [("QP", i)])
                memset("pool", KP[i][64:70, :, :], 1.0, [("KP", i)])

            for hp in range(4):
                i = hp % 2
                VT = VTB[i]
                vk_ = ("VTB", i)
                chains = [(j, tg, dst, dk, gain) for j, (dst, dk, gain) in enumerate(((QP[i], ("QP", i), gq_b), (KP[i], ("KP", i), gk_b)))
                          for tg in range(4)]
                qk_chains(i, chains, False)
                dma("sync", QP[i][64:67, :, :], cps[1, :, 2 * hp:2 * hp + 2, :], [("cps", 1, j_) for j_ in range(3)], [("QP", i)])
                dma("sync", KP[i][67:70, :, :], cps[0, :, 2 * hp:2 * hp + 2, :], [("cps", 0, j_) for j_ in range(3)], [("KP", i)])
                v_proj(i, VT, vk_)
                vaug_build(VT, vk_, VAB[i], ("VAB", i), [VT[:, 128 * t:128 * (t + 1)] for t in range(NT)], [3, 4, 6, 7])
                if hp < 3:
                    load_wsl((hp + 1) % 2, 1536 + 128 * (hp + 1), 2048 + 128 * (hp + 1), 2560 + 128 * (hp + 1))
                if debug_stage == 3 and hp == 0:
                    dbg_out("QP", QP[i][0:70, :, :], [70, 2, S], [("QP", i)], BF16)
                    dbg_out("KP", KP[i][0:70, :, :], [70, 2, S], [("KP", i)], BF16)

                steps = []
                for n in range(4):
                    for hh in range(2):
                        u = new_unit(4 + hp, hh, n)
                        Ob = u["Ob"]
                        Kh = KP[i][0:70, hh, :]
                        Qh = QP[i][0:70, hh, :]
                        for jb in range(4 * n + 4):
                            jj = jb - 4 * n
                            c0 = 0 if jj < 0 else 128 * jj
                            steps.append(dict(u=u, qk_keys=[("KP", i), ("QP", i)], c0=c0, c1=512,
                                              mask=(None if jj < 0 else maskC), mcols=(c0, c0 + 128), meng="dve",
                                              last=(jb == 4 * n + 3),
                                              qk_list=[(c0, 512, Kh[:, 128 * jb:128 * (jb + 1)], Qh[:, 512 * n + c0:512 * (n + 1)])],
                                              pv_list=[(bank(Ob)[:, c0:512], VAB[i][:, jb, hh, :], ("VAB", i), c0, 512)]))
                run_pipeline(steps)

            MIXK = [("MIXT", c, n) for c in range(8) for n in range(4)]
            if debug_stage == 3:
                dbg_out("MIXT", MIXT, [128, 8, S], MIXK, BF16)
                raise _Stop()

            P.barrier()
            WO = A.v(O_W, [128, 8, D])
            dma("pool", WO, w_o.rearrange("(k p) n -> p k n", p=128), (), ["WO"])
            WR = A.v(O_TB, [128, 8, 36], F32)
            dma("sync", WR, w_r.rearrange("(k p) n -> p k n", p=128), (), ["WR"])
            dma("sync", gbc, dram_pbcast(g_ffn, 128, D), (), ["gbc"])
            M1all = A.v(O_TB + 2048, [128, NT, 32], F32)
            gw = rt[:, 0:1]; gmax = rt[:, 1:2]; ngmax = rt[:, 2:3]; sg = rt[:, 3:4]
            m1 = rt[:, 4:5]; m2 = rt[:, 5:6]; dd = rt[:, 6:7]; w1 = rt[:, 7:8]; w2 = rt[:, 8:9]
            ohg = rt[:, 12:16]; eg = rt[:, 16:20]; pen = rt[:, 20:24]
            g1 = rt[:, 24:56]
            RK = ["rt"]
            m32b = [m32, A.v(O_TB + 20480, [128, 8, 128], F32)]

            def s3A(t):
                i = t % 2
                dma("sync", xt[i], x[t * 128:(t + 1) * 128, :], (), [("xt", i)])
                for half in range(2):
                    pk = ("ps", half)
                    for c in range(8):
                        mm(bank(half), MIXT[:, c, t * 128:(t + 1) * 128], WO[:, c, half * 512:(half + 1) * 512], c == 0, c == 7,
                           ["WO"] + [("MIXT", c, t // 4)], [pk])
                    tt("dve", h[:, t, half * 512:(half + 1) * 512], bank(half), xt[i][:, half * 512:(half + 1) * 512], ALU.add,
                       [pk, ("xt", i)], [("h", t)])

            def s3B(t):
                norm_stats(h[:, t, :], ("h", t), t % 2)

            def s3C(t):
                i = t % 2
                norm_tr(t, i, 2 + 2 * i, m32b[i], ("m32", i))
                if SPARSE:
                    dma("pool", mrows[t * 128:(t + 1) * 128, :], hn[i], [("hn", i)], [("mrows", t)])

            def s3D(t):
                rk = ("ps", 6)
                for kc in range(8):
                    mm(bank(6)[:, 0:36], m32b[t % 2][:, kc, :], WR[:, kc, :], kc == 0, kc == 7, [("m32", t % 2), "WR"], [rk])
                tt("dve", L_r, bank(6)[:, 0:36], bias_r, ALU.add, [rk, "sm_br"], ["L"])
                red(gmax, L_r[:, 0:4], ALU.max, ["L"], RK)
                ts("dve", ohg, L_r[:, 0:4], gmax, None, ALU.is_equal, None, ["L"] + RK, RK)
                ts("dve", ngmax, gmax, -1.0, None, ALU.mult, None, RK, RK)
                act(eg, L_r[:, 0:4], AF.Exp, ["L"] + RK, RK, bias=ngmax, scale=1.0)
                red(sg, eg, ALU.add, RK, RK)
                recip(gw, sg, RK, RK)
                ts("dve", pen, ohg, -1.0, BIG, ALU.add, ALU.mult, RK, RK)
                lem = L_r[:, 4:36]
                tt("dve", lem.rearrange("p (g e) -> p g e", g=4), lem.rearrange("p (g e) -> p g e", g=4), bcast_last(pen, 8),
                   ALU.add, ["L"] + RK, ["L"])
                red(m1, lem, ALU.max, ["L"], RK)
                ts("dve", msk1, lem, m1, None, ALU.is_equal, None, ["L"] + RK, ["msk1"])
                stt(lem2, msk1, -BIG, lem, ALU.mult, ALU.add, ["msk1", "L"], ["lem2"])
                red(m2, lem2, ALU.max, ["lem2"], RK)
                ts("dve", lem2, lem2, m2, None, ALU.is_equal, None, ["lem2"] + RK, ["lem2"])
                tt("dve", dd, m2, m1, ALU.subtract, RK, RK)
                act(dd, dd, AF.Exp, RK, RK)
                ts("dve", dd, dd, 1.0, None, ALU.add, None, RK, RK)
                recip(w1, dd, RK, RK)
                tt("dve", w1, w1, gw, ALU.mult, RK, RK)
                tt("dve", w2, gw, w1, ALU.subtract, RK, RK)
                ts("dve", g1, msk1, w1, None, ALU.mult, None, ["msk1"] + RK, RK)
                stt(gate_t[:, t, :], lem2, w2, g1, ALU.mult, ALU.add, ["lem2"] + RK, [("gate", t)])
                if SPARSE:
                    cp("pool", M2all[:, t, :], lem2, ["lem2"], [("M2", t)])
                    cp("pool", M1all[:, t, :], msk1, ["msk1"], [("M1", t)])
                    cp("dve", W12[:, 0, t:t + 1], w1, RK, [("W12", t)])
                    cp("dve", W12[:, 1, t:t + 1], w2, RK, [("W12", t)])


            skew([s3A, s3B, s3C, s3D], NT)

            HK = [("h", t) for t in range(NT)]
            GK = [("gate", t) for t in range(NT)]
            if debug_stage == 4:
                dbg_out("h1", h, [128, NT, D], HK)
                dbg_out("gate", gate_t[:, :, :], [128, NT, 32], GK)
                dbg_out("XT2", XT, [128, 8, S], XTK, BF16)
                raise _Stop()

            if SPARSE:
                pidx = cvec[:, 0:1]
                svals = cvec[:, 1:65]
                TS = A.v(O_TB + 4096, [128, NT, 32], F32)
                BASE = A.v(O_TB + 6144, [128, NT, 32], F32)
                SLF = A.v(O_TB + 8192, [128, NT, 32], F32)
                TMPS = A.v(O_TB + 10240, [128, NT, 32], F32)
                MB = A.v(O_TB + 12288, [128, NT * 32])
                CMP = A.v(O_TB + 14336, [128, NSLOT, 32], F32)
                CT = rt
                M1K = [("M1", t) for t in range(NT)]
                M2K = [("M2", t) for t in range(NT)]
                tt("dve", MB.rearrange("p (a b) -> p a b", a=NT), M1all, M2all[:, :, :], ALU.add, M1K + M2K, ["MB"])
                mm(bank(0), onesm[:, :], MB, True, True, ["MB", "onesm"], [("ps", 0)])
                mm(bank(1), LST, MB, True, True, ["MB", "cmat"], [("ps", 1)])
                cp("act", TS, bank(0).rearrange("p (a b) -> p a b", a=NT), [("ps", 0)], ["TS"])
                memset("pool", BASE[:, 0, :], 0.0, ["BASE"])
                for j in range(1, NT):
                    tt("dve", BASE[:, j, :], BASE[:, j - 1, :], TS[:, j - 1, :], ALU.add, ["BASE", "TS"], ["BASE"])
                cnt_e = CT[:, 0:32]
                yv = smallf[:, 176:208]
                kfv = smallf[:, 208:240]
                kiv = A.v(O_TB + 1152, [128, 32], I32)
                tt("dve", cnt_e, BASE[:, NT - 1, :], TS[:, NT - 1, :], ALU.add, ["BASE", "TS"], ["cnt"])
                ts("dve", yv, cnt_e, float(CAP - 1), 1.0 / CAP, ALU.add, ALU.mult, ["cnt"], ["yv"])
                cp("dve", kiv, yv, ["yv"], ["kiv"])
                cp("dve", kfv, kiv, ["kiv"], ["kfv"])
                tt("dve", cnt_e, kfv, yv, ALU.is_gt, ["kfv", "yv"], ["cnt"])
                tt("dve", kfv, kfv, cnt_e, ALU.subtract, ["kfv", "cnt"], ["kfv"])
                oc2 = smallf[:, 10:11]
                ones32 = bass.AP(oc2.tensor, oc2.offset, [list(oc2.ap[0]), [0, 32]])
                memset("pool", smallf[:, 10:11], 1.0, ["ones8"])
                cum = CT[:, 32:64]
                P.op("dve", lambda e: e.tensor_tensor_scan(out=cum, data0=ones32, data1=kfv, initial=0.0, op0=ALU.mult, op1=ALU.add),
                     ["ones8", "kfv"], ["cum"])
                offv = yv
                tt("dve", offv, cum, kfv, ALU.subtract, ["cum", "kfv"], ["yv"])
                ts("dve", offv, offv, float(CAP), None, ALU.mult, None, ["yv"], ["yv"])
                tt("dve", SLF, bank(1).rearrange("p (a b) -> p a b", a=NT), BASE, ALU.add, [("ps", 1), "BASE"], ["SLF"])
                tt("dve", SLF, SLF, bcast_mid(offv, NT), ALU.add, ["SLF", "yv"], ["SLF"])
                S12f = CT[:, 0:32].rearrange("p (a b) -> p a b", a=2)
                tt("dve", TMPS, SLF, M1all, ALU.mult, ["SLF"] + M1K, ["TMPS"])
                red(S12f[:, 0, :], TMPS, ALU.add, ["TMPS"], ["S12f"])
                tt("dve", TMPS, SLF, M2all[:, :, :], ALU.mult, ["SLF"] + M2K, ["TMPS"])
                red(S12f[:, 1, :], TMPS, ALU.add, ["TMPS"], ["S12f"])
                cp("dve", S12i[:, :, :], S12f, ["S12f"], ["S12i"])
                tt("dve", CMP, bcast_mid(cum, NSLOT), bcast_last(svals[:, 0:NSLOT], 32), ALU.is_le, ["cum", "cvec"], ["CMP"])
                es = A.v(O_TB + 1280, [128, NSLOT], F32)
                red(es, CMP, ALU.add, ["CMP"], ["es"])
                ts("dve", es, es, 128.0, None, ALU.mult, None, ["es"], ["es"])
                ts("dve", es, es, pidx, None, ALU.add, None, ["es", "cvec"], ["es"])
                for q in range(4):
                    ts("dve", idxU[:, :, q], es, 4.0, float(q), ALU.mult, ALU.add, ["es"], [("idxU", q)])
                bigf = A.v(O_TB + 1792, [128, NSLOT], F32)
                ts("dve", bigf, es, float(N_EXP * 128), 20000.0, ALU.is_ge, ALU.mult, ["es"], ["bigf"])
                for q in range(2):
                    stt(idxD[:, :, q], es, 2.0, bigf, ALU.mult, ALU.add, ["es", "bigf"], [("idxD", q)])
                    if q:
                        ts("dve", idxD[:, :, q], idxD[:, :, q], 1, None, ALU.add, None, [("idxD", q)], [("idxD", q)])
                IDXK = [("idxU", q) for q in range(4)] + [("idxD", q) for q in range(2)]
                ZT = A.v(O_TA, [128, NSUB * 16], I32)
                memset("pool", ZT, 0, ["ZT"])
                dma("sync", tos.rearrange("(p a) b -> p (a b)", p=128), ZT, ["ZT"], ["tos"])
                tokf = A.v(O_TB + 1536, [128, NT], F32)
                ts("dve", tokf, cvec[:, 65:81], pidx, None, ALU.add, None, ["cvec"], ["tokf"])
                cp("dve", tokrep[:, :, :], bcast_last(tokf, 16), ["tokf"], ["tokrep"])
                for j in range(NT):
                    for k in range(2):
                        P.dma("pool", lambda e, j=j, k=k: e.indirect_dma_start(
                            out=tos, out_offset=bass.IndirectOffsetOnAxis(ap=S12i[:, k, j:j + 1], axis=0),
                            in_=tokrep[:, j, :], in_offset=None), ["S12i", "tokrep", "tos"], [("tosw", j, k)])
                TOSK = [("tosw", j, k) for j in range(NT) for k in range(2)]
                if debug_stage == 6:
                    dbg_out("S12", S12i[:, :, :], [128, 2, NT], ["S12i"], I32)
                    dbg_out("idxU", idxU[:, :, :], [128, NSLOT, 4], IDXK, I32)
                    dbg_out("cum", cum, [128, 32], ["cum"])
                    TOKD = A.v(O_TA + 8192, [128, NSUB, 16], I32)
                    dma("sync", TOKD, tos.rearrange("(s q) r -> q s r", q=128), TOSK, ["TOKD"])
                    dbg_out("tos", TOKD, [128, NSUB, 16], ["TOKD"], I32)
                    raise _Stop()

                P.barrier()
                NB = 3
                WU = [A.v(O_MIX, [128, 8, 1024]), A.v(O_MIX + 16384, [128, 8, 1024]), A.v(O_XT, [128, 8, 1024])]
                WD = [A.v(O_W, [128, 4, D]), A.v(O_W + 8192, [128, 4, D]), A.v(O_XT + 16384, [128, 4, D])]
                XGa = [O_TA, O_TA + 4096, O_XT + 24576]
                XG = [[A.v(XGa[i] + 2048 * u, [128, D]) for u in range(2)] for i in range(NB)]
                XGT = [A.v(O_TA + 8192 + 4096 * i, [128, 8, 256]) for i in range(2)]
                HIDs = [A.v(O_TB + 2048 * i, [128, 4, 256]) for i in range(2)]
                SAs = [A.v(O_TB + 4096 + 2048 * i, [128, 1024]) for i in range(2)]
                YS = [A.v(O_TB + 8192 + 4096 * i, [128, D], F32) for i in range(2)]
                n_slot_run = NTILE_RUN if debug_stage != 7 else 4
                ysc = [0]
                P.dma("sync", lambda e: e.dma_start(out=TOK[:, :], in_=tos.rearrange("(s q) r -> q s r", q=128)[:, :, 0],
                                                    allow_slow_non_contiguous=True), (), ["TOK"])

                lo_, hi_ = 0, n_slot_run - 1
                order = []
                while lo_ <= hi_:
                    order.append(lo_)
                    lo_ += 1
                    if lo_ <= hi_ and len(order) % 2 == 1 and hi_ >= 32:
                        order.append(hi_)
                        hi_ -= 1

                def bufi(k):
                    return (k // 2) % 2 if k % 2 == 0 else 2

                def slot_load(k):
                    i = bufi(k)
                    sl = order[k]
                    for u in range(2):
                        ss = 2 * sl + u
                        P.dma("pool", lambda e, ss=ss, u=u: e.indirect_dma_start(
                            out=XG[i][u], out_offset=None, in_=mrows, in_offset=bass.IndirectOffsetOnAxis(ap=TOK[:, ss:ss + 1], axis=0)),
                            ["TOK"], [("XG", i, u)])
                    for q in range(4):
                        P.dma("pool", lambda e, q=q: e.indirect_dma_start(
                            out=WU[i][:, 2 * q:2 * q + 2, :].rearrange("p a b -> p (a b)"), out_offset=None, in_=w_up_r,
                            in_offset=bass.IndirectOffsetOnAxis(ap=idxU[:, sl, q:q + 1], axis=0),
                            bounds_check=bnd_reg, oob_is_err=False), (), [("WU", i, q)])
                    for q in range(2):
                        P.dma("pool", lambda e, q=q: e.indirect_dma_start(
                            out=WD[i][:, 2 * q:2 * q + 2, :].rearrange("p a b -> p (a b)"), out_offset=None, in_=w_down_r,
                            in_offset=bass.IndirectOffsetOnAxis(ap=idxD[:, sl, q:q + 1], axis=0),
                            bounds_check=bnd_reg, oob_is_err=False), (), [("WD", i, q)])

                def tile_T(k):
                    i = bufi(k)
                    j2 = k % 2
                    for u in range(2):
                        for kc in range(8):
                            mm(psum[:, kc * 128:(kc + 1) * 128], XG[i][u][:, kc * 128:(kc + 1) * 128], identb, True, True,
                               [("XG", i, u), "cmat"], [("ps", kc // 4)], inc=(kc % 4 == 3))
                        pv_ = bank(0, 2).rearrange("p (a b) -> p a b", a=8)
                        cp("act", XGT[j2][:, 0:4, u * 128:(u + 1) * 128], pv_[:, 0:4, :], [("ps", 0)], [("XGT", j2)])
                        cp("dve", XGT[j2][:, 4:8, u * 128:(u + 1) * 128], pv_[:, 4:8, :], [("ps", 1)], [("XGT", j2)])

                def tile_U(k):
                    i = bufi(k)
                    j2 = k % 2
                    for part in range(2):
                        for fc in range(4):
                            bnk = 2 + 2 * part + fc // 2
                            pk = ("ps", bnk)
                            fcol = fc + 4 * part
                            c0 = (fc % 2) * 256
                            for kc in range(8):
                                mm(bank(bnk)[:, c0:c0 + 256], WU[i][:, kc, fcol * 128:(fcol + 1) * 128], XGT[j2][:, kc, :],
                                   kc == 0, kc == 7, [("WU", i, kc // 2), ("XGT", j2)], [pk], inc=(kc == 7 and fc % 2 == 1))
                    act(SAs[j2], bank(2, 2), AF.Silu, [("ps", 2), ("ps", 3)], [("SAs", j2)])
                    tt("dve", HIDs[j2].rearrange("p a b -> p (a b)"), SAs[j2], bank(4, 2), ALU.mult, [("SAs", j2), ("ps", 4), ("ps", 5)], [("HIDs", j2)])

                def tile_D(k):
                    i = bufi(k)
                    j2 = k % 2
                    sl = order[k]
                    for u in range(2):
                        yi = ysc[0] % 2
                        ysc[0] += 1
                        for half in range(2):
                            pk = ("ps", 6 + half)
                            for fc in range(4):
                                mm(bank(6 + half), HIDs[j2][:, fc, u * 128:(u + 1) * 128], WD[i][:, fc, half * 512:(half + 1) * 512], fc == 0, fc == 3,
                                   [("HIDs", j2), ("WD", i, fc // 2)], [pk])
                            cp("act" if half == 0 else "dve", YS[yi][:, half * 512:(half + 1) * 512], bank(6 + half), [pk], [("YS", yi)])
                        ss = 2 * sl + u
                        dma("act", Yd[ss * 128:(ss + 1) * 128, :], YS[yi], [("YS", yi)], [("Yd", ss)])

                for k0 in range(min(3, n_slot_run)):
                    slot_load(k0)
                for sl in range(n_slot_run + 1):
                    if sl < n_slot_run:
                        tile_T(sl)
                    if sl >= 1:
                        tile_D(sl - 1)
                    if sl < n_slot_run:
                        tile_U(sl)
                    nxt = sl + 3 if sl % 2 == 1 else sl + 1
                    if sl >= 1 and 3 <= nxt < n_slot_run:
                        slot_load(nxt)

                YG = [[A.v(O_TA + 8192 * i + 4096 * k, [128, D], F32) for k in range(2)] for i in range(2)]
                YDK = [("Yd", ss) for ss in range(2 * n_slot_run)]
                for t in range(NT):
                    i = t % 2
                    for k in range(2):
                        P.dma("pool", lambda e, k=k, t=t, i=i: e.indirect_dma_start(
                            out=YG[i][k], out_offset=None, in_=Yd, in_offset=bass.IndirectOffsetOnAxis(ap=S12i[:, k, t:t + 1], axis=0)),
                            YDK, [("YG", i, k)])
                        stt(h[:, t, :], YG[i][k], W12[:, k, t:t + 1], h[:, t, :], ALU.mult, ALU.add, [("YG", i, k), ("h", t)], [("h", t)])
                if debug_stage in (5, 7):
                    dbg_out("h2", h, [128, NT, D], HK)
                    raise _Stop()

            if not SPARSE:
                P.barrier()
                WU = [A.v(O_MIX + 16384 * i, [128, 8, 1024]) for i in range(2)]
                WD = [A.v(O_W + 8192 * i, [128, 4, D]) for i in range(2)]
                HID = [A.v(O_TB + 4096 * i, [128, 4, 512]) for i in range(2)]
                SA = [A.v(O_TB + 8192 + 1024 * i, [128, 512]) for i in range(2)]
                n_exp_run = N_EXP if debug_stage != 5 else 2
                ub = 0
                db = 0
                sai = 0
                for ex in range(n_exp_run):
                    wi = ex % 2
                    dma("pool", WU[wi], w_up[ex].rearrange("(k p) n -> p k n", p=128), (), [("WU", wi)])
                    dma("pool", WD[wi], w_down[ex].rearrange("(k p) n -> p k n", p=128), (), [("WD", wi)])
                    for tg in range(4):
                        hi_ = (ex * 4 + tg) % 2
                        hk = ("HID", hi_)
                        for fc in range(4):
                            b0 = 2 * (ub % 2); ub += 1
                            for part, fcol in enumerate((fc, fc + 4)):
                                pk = ("ps", b0 + part)
                                for kc in range(8):
                                    mm(bank(b0 + part), WU[wi][:, kc, fcol * 128:(fcol + 1) * 128], XT[:, kc, tg * 512:(tg + 1) * 512],
                                       kc == 0, kc == 7, [("WU", wi)] + XTK[tg * 4:(tg + 1) * 4], [pk])
                            s_ = sai % 2; sai += 1
                            act(SA[s_], bank(b0), AF.Silu, [("ps", b0)], [("SA", s_)])
                            tt("dve", HID[hi_][:, fc, :], SA[s_], bank(b0 + 1), ALU.mult, [("SA", s_), ("ps", b0 + 1)], [hk])
                        for tt_ in range(4):
                            t = 4 * tg + tt_
                            for half in range(2):
                                bnk = 4 + (db % 4); db += 1
                                pk = ("ps", bnk)
                                for fc in range(4):
                                    mm(bank(bnk), HID[hi_][:, fc, tt_ * 128:(tt_ + 1) * 128], WD[wi][:, fc, half * 512:(half + 1) * 512],
                                       fc == 0, fc == 3, [hk, ("WD", wi)], [pk])
                                hv = h[:, t, half * 512:(half + 1) * 512]
                                stt(hv, bank(bnk), gate_t[:, t, ex:ex + 1], hv, ALU.mult, ALU.add, [pk, ("gate", t), ("h", t)], [("h", t)])

                if debug_stage == 5:
                    dbg_out("h2", h, [128, NT, D], HK)
                    raise _Stop()

            P.barrier()
            WG = A.v(O_W, [128, 8, D])
            WP = A.v(O_MIX, [128, 2, D])
            PTT = A.v(O_MIX + 4096, [128, 2, S])
            SG = [A.v(O_TB + 4096 * i, [128, D], F32) for i in range(2)]
            TM = [A.v(O_TB + 8192 + 4096 * i, [128, D], F32) for i in range(2)]
            ptile = [A.v(O_TB + 16384 + 1024 * i, [128, 256], F32) for i in range(2)]
            dma("pool", WG, w_pg.rearrange("(k p) n -> p k n", p=128), (), ["WG"])
            dma("pool", WP, w_pp.rearrange("(k p) n -> p k n", p=128), (), ["WP"])
            dma("sync", gbc, dram_pbcast(g_ple, 128, D), (), ["gbc"])
            def s5A(t):
                i = t % 2
                dma("sync", ptile[i], p_in[t * 128:(t + 1) * 128, :], (), [("ptile", i)])
                norm_stats(h[:, t, :], ("h", t), i)

            def s5B(t):
                i = t % 2
                norm_tr(t, i, 0)
                pk6 = ("ps", 6)
                for c in range(2):
                    tr32(bank(6)[:, c * 128:(c + 1) * 128], ptile[i][:, c * 128:(c + 1) * 128], [("ptile", i), "ident"], [pk6], inc=(c == 1))
                cp("act", PTT[:, :, t * 128:(t + 1) * 128], bank(6)[:, 0:256].rearrange("p (a b) -> p a b", a=2), [pk6], [("PTT", t)])

            def s5C(t):
                i = t % 2
                for half in range(2):
                    gk_ = ("ps", 2 + half)
                    for kc in range(8):
                        mm(bank(2 + half), XT[:, kc, t * 128:(t + 1) * 128], WG[:, kc, half * 512:(half + 1) * 512], kc == 0, kc == 7,
                           ["WG", ("XT", t)], [gk_])
                    ppk = ("ps", 4 + half)
                    for c in range(2):
                        mm(bank(4 + half), PTT[:, c, t * 128:(t + 1) * 128], WP[:, c, half * 512:(half + 1) * 512], c == 0, c == 1,
                           ["WP", ("PTT", t)], [ppk])
                    hs = slice(half * 512, (half + 1) * 512)
                    act(SG[i][:, hs], bank(2 + half), AF.Sigmoid, [gk_], [("SG", i, half)])
                    tt("dve", TM[i][:, hs], SG[i][:, hs], bank(4 + half), ALU.mult, [("SG", i, half), ppk], [("TM", i, half)])
                    tt("pool", TM[i][:, hs], TM[i][:, hs], h[:, t, hs], ALU.add, [("TM", i, half), ("h", t)], [("TM", i, half)])
                dma("sync", out[t * 128:(t + 1) * 128, :], TM[i], [("TM", i, 0), ("TM", i, 1)], (), is_output=True)

            skew([s5A, s5B, s5C], NT)


        try:
            body()
        except _Stop:
            pass
        P.finish()
        P.emit()
        n_ops = P.n_ops
    return nc, dbg, n_ops


def make_consts():
    cm = np.zeros((128, NCM), np.float32)
    cm[:, 0:128] = np.eye(128, dtype=np.float32)
    bd = np.zeros((128, 128), np.float32)
    bd[0:64, 0:64] = 1.0
    bd[64:128, 64:128] = 1.0
    cm[:, 128:256] = bd
    rot = np.zeros((128, 128), np.float32)
    for hb in (0, 64):
        for i in range(8):
            rot[hb + i + 8, hb + i] = -1.0
            rot[hb + i, hb + i + 8] = 1.0
    cm[:, 256:384] = rot
    b = np.arange(128)[:, None]
    a = np.arange(128)[None, :]
    cm[:, 384:512] = (b <= a).astype(np.float32)
    cm[:, 512:640] = (b >= a).astype(np.float32)
    for n in range(4):
        a3 = np.arange(32)[None, :]
        cm[:, 640 + 32 * n:672 + 32 * n] = (b <= 32 * n + a3).astype(np.float32)
    cm[:, 768:896] = (b < a).astype(np.float32)
    cvec = np.zeros((128, 96), np.float32)
    cvec[:, 0] = np.arange(128)
    cvec[:, 1:65] = np.arange(64)[None, :]
    cvec[:, 65:81] = 128.0 * np.arange(16)[None, :]
    ident = np.eye(128, dtype=np.float32)
    inv = (np.float32(500000.0) ** (-np.arange(0, 16, 2, dtype=np.float32) / np.float32(16))).astype(np.float32)
    invf = np.zeros((128, 1), np.float32)
    for p in range(128):
        d = p % 64
        if d < 16:
            invf[p, 0] = inv[d % 8]
    return cm, ident, invf, cvec


_CACHE = {}


def _get_program(debug_stage=None):
    if debug_stage not in _CACHE:
        _CACHE[debug_stage] = build_program(debug_stage)
    return _CACHE[debug_stage]


def make_in_maps(x, p, positions, g_mix, w_in, b_f, qn_a, kn_a, qn_b, kn_b, w_o, g_ffn, w_rg, b_rg, w_re, b_re,
                 w_up, w_down, g_ple, w_ple_gate, w_ple_proj, n_cores=8):
    f = lambda a: np.ascontiguousarray(np.asarray(a), dtype=np.float32)
    cm, ident, invf, cvec = make_consts()
    w_r = np.concatenate([f(w_rg)[0], f(w_re)[0].transpose(1, 0, 2).reshape(D, 32)], axis=1)
    b_r = np.concatenate([f(b_rg)[0].reshape(4), f(b_re)[0].reshape(32)])
    shared = {
        "g_mix": f(g_mix)[0], "w_in": f(w_in)[0], "b_f": f(b_f)[0],
        "qn_a": f(qn_a)[0], "kn_a": f(kn_a)[0], "qn_b": f(qn_b)[0], "kn_b": f(kn_b)[0],
        "w_o": f(w_o)[0], "g_ffn": f(g_ffn)[0], "w_r": np.ascontiguousarray(w_r), "b_r": np.ascontiguousarray(b_r),
        "w_up_r": np.ascontiguousarray(f(w_up)[0].reshape(N_EXP, 8, 128, 1024).transpose(0, 2, 1, 3)).reshape(N_EXP * 128 * 4, 2048),
        "w_down_r": np.ascontiguousarray(f(w_down)[0].reshape(N_EXP, 4, 128, D).transpose(0, 2, 1, 3)).reshape(N_EXP * 128 * 2, 2048),
        "g_ple": f(g_ple)[0],
        "w_pg": f(w_ple_gate)[0], "w_pp": f(w_ple_proj)[0],
        "cmat": cm, "ident": ident, "invf": invf, "cvec": cvec,
    }
    xs = f(x)
    ps = f(p)[0]
    pos = np.ascontiguousarray(np.asarray(positions), dtype=np.int32)
    in_maps = []
    for b in range(n_cores):
        m = dict(shared)
        m["x"] = xs[b]
        m["p"] = ps[b]
        m["pos"] = pos[b]
        in_maps.append(m)
    return in_maps


def kernel(**inputs):
    nc, _, _ = _get_program(None)
    in_maps = make_in_maps(**inputs)
    res = run_bass_kernel_spmd(nc, in_maps, core_ids=list(range(8)))
    return np.stack([np.asarray(r["out"]) for r in res.results], axis=0).astype(np.float32)
```
